# Optimizing a Trainium2 kernel written in Bass

```python
import jax, jax.numpy as jnp
from jax import lax
import numpy as np


D_MODEL = 1024
BATCH = 4
SEQ = 4096
DEPTH = 2

GRID_W = 64
CTX_LEN = 256
EPS = 1e-6

N_HEADS = 8
N_KV_HEADS = 2
HEAD_DIM = 64
GQA_GROUP = N_HEADS // N_KV_HEADS
WINDOW = 128
ATTN_BLOCK = 128
ROPE_BASE = 10000.0
ROPE_PAIRS = HEAD_DIM // 4

CONV_CH = 512
CONV_K = 31
CONV_PAD = CONV_K // 2

POOL_WINDOWS = (2, 4, 8, 16)
POOL_GROUP = 128
POOL_CH = POOL_GROUP * len(POOL_WINDOWS)

N_EXPERTS = 16
CAPACITY_FACTOR = 2
D_EXPERT = 1024

Q_W = N_HEADS * HEAD_DIM
KV_W = N_KV_HEADS * HEAD_DIM
N_BRANCHES = 3
IN_OFFSETS = (Q_W, Q_W + KV_W, Q_W + 2 * KV_W, Q_W + 2 * KV_W + 2 * CONV_CH,
              Q_W + 2 * KV_W + 2 * CONV_CH + POOL_CH)
IN_W = IN_OFFSETS[-1] + N_BRANCHES * D_MODEL

kernel_name = 'hybrid_gated_branch_dit_block'

F32 = jnp.float32


def rmsnorm(x, g):
    xf = x.astype(F32)
    y = xf * lax.rsqrt(jnp.mean(xf * xf, axis=-1, keepdims=True) + EPS)
    return (y * g.astype(F32)).astype(x.dtype)


def layernorm(x, g, b):
    xf = x.astype(F32)
    mu = jnp.mean(xf, axis=-1, keepdims=True)
    var = jnp.mean(jnp.square(xf - mu), axis=-1, keepdims=True)
    return ((xf - mu) * lax.rsqrt(var + EPS) * g.astype(F32) + b.astype(F32)).astype(x.dtype)


def modulate(h, shift, scale):
    return h * (1 + scale) + shift


def rope_tables(n):
    rows = n // GRID_W
    row = jnp.broadcast_to(jnp.arange(rows)[:, None], (rows, GRID_W)).reshape(-1).astype(F32)
    col = jnp.broadcast_to(jnp.arange(GRID_W)[None, :], (rows, GRID_W)).reshape(-1).astype(F32)
    freqs = ROPE_BASE ** (-jnp.arange(ROPE_PAIRS, dtype=F32) / ROPE_PAIRS)
    ang_r = row[:, None] * freqs
    ang_c = col[:, None] * freqs
    return (jnp.cos(ang_r), jnp.sin(ang_r), jnp.cos(ang_c), jnp.sin(ang_c))


def _rotate(x1, x2, cos, sin):
    return x1 * cos - x2 * sin, x2 * cos + x1 * sin


def apply_axial_rope(x, tabs):
    cos_r, sin_r, cos_c, sin_c = [t[:, None, :].astype(x.dtype) for t in tabs]
    xr1, xr2, xc1, xc2 = jnp.split(x, 4, axis=-1)
    r1, r2 = _rotate(xr1, xr2, cos_r, sin_r)
    c1, c2 = _rotate(xc1, xc2, cos_c, sin_c)
    return jnp.concatenate([r1, r2, c1, c2], axis=-1)


def heads(z, n):
    return z.reshape(z.shape[0], z.shape[1], n, HEAD_DIM)


def window_attention(q, k, v, k_ctx, v_ctx, sink):
    B, S = q.shape[:2]
    L = k_ctx.shape[1]
    nb = S // ATTN_BLOCK
    qb = q.reshape(B, nb, ATTN_BLOCK, N_KV_HEADS, GQA_GROUP, HEAD_DIM) * (HEAD_DIM ** -0.5)
    pad = ((0, 0), (1, 1), (0, 0), (0, 0), (0, 0))
    kp = jnp.pad(k.reshape(B, nb, ATTN_BLOCK, N_KV_HEADS, HEAD_DIM), pad)
    vp = jnp.pad(v.reshape(B, nb, ATTN_BLOCK, N_KV_HEADS, HEAD_DIM), pad)
    kw = jnp.concatenate([kp[:, :-2], kp[:, 1:-1], kp[:, 2:]], axis=2)
    vw = jnp.concatenate([vp[:, :-2], vp[:, 1:-1], vp[:, 2:]], axis=2)
    s_win = jnp.einsum('bnqkgd,bnskd->bnkgqs', qb, kw).astype(F32)
    qi = jnp.arange(ATTN_BLOCK)[:, None]
    kj = jnp.arange(3 * ATTN_BLOCK)[None, :]
    band = jnp.abs(kj - ATTN_BLOCK - qi) <= WINDOW
    kpos = (jnp.arange(nb)[:, None, None] - 1) * ATTN_BLOCK + kj[None]
    mask = band[None] & (kpos >= 0) & (kpos < S)
    s_win = jnp.where(mask[None, :, None, None], s_win, -1e30)
    s_ctx = jnp.einsum('bnqkgd,blkd->bnkgql', qb, k_ctx).astype(F32)
    sink_col = jnp.broadcast_to(sink.reshape(N_KV_HEADS, GQA_GROUP, 1, 1).astype(F32),
                                (B, nb, N_KV_HEADS, GQA_GROUP, ATTN_BLOCK, 1))
    p = jax.nn.softmax(jnp.concatenate([s_win, s_ctx, sink_col], axis=-1), axis=-1)
    p_win = p[..., :3 * ATTN_BLOCK].astype(v.dtype)
    p_ctx = p[..., 3 * ATTN_BLOCK:3 * ATTN_BLOCK + L].astype(v.dtype)
    o = jnp.einsum('bnkgqs,bnskd->bnqkgd', p_win, vw) + jnp.einsum('bnkgql,blkd->bnqkgd', p_ctx, v_ctx)
    return o.reshape(B, S, Q_W)


def context_attention(q, k, v, sink):
    B, L = q.shape[:2]
    qg = q.reshape(B, L, N_KV_HEADS, GQA_GROUP, HEAD_DIM) * (HEAD_DIM ** -0.5)
    s = jnp.einsum('blkgd,bmkd->bkglm', qg, k).astype(F32)
    sink_col = jnp.broadcast_to(sink.reshape(N_KV_HEADS, GQA_GROUP, 1, 1).astype(F32),
                                (B, N_KV_HEADS, GQA_GROUP, L, 1))
    p = jax.nn.softmax(jnp.concatenate([s, sink_col], axis=-1), axis=-1)[..., :L].astype(v.dtype)
    o = jnp.einsum('bkglm,bmkd->blkgd', p, v)
    return o.reshape(B, L, Q_W)


def conformer_conv(z, w_dw, b_dw, ln_g, ln_b, w_o):
    a, gl = jnp.split(z, 2, axis=-1)
    u = a * jax.nn.sigmoid(gl)
    u = lax.conv_general_dilated(u, w_dw[:, None, :], window_strides=(1,),
                                 padding=((CONV_PAD, CONV_PAD),),
                                 dimension_numbers=('NWC', 'WIO', 'NWC'),
                                 feature_group_count=CONV_CH) + b_dw
    u = jax.nn.silu(layernorm(u, ln_g, ln_b))
    return u @ w_o


def multiscale_pool(z, w_pool, pool_scale):
    B, N, C = z.shape
    cs = jnp.concatenate([jnp.zeros((B, 1, C), F32), jnp.cumsum(z.astype(F32), axis=1)], axis=1)
    t = jnp.arange(N)
    outs = []
    for gi, w in enumerate(POOL_WINDOWS):
        sl = slice(gi * POOL_GROUP, (gi + 1) * POOL_GROUP)
        lo = jnp.clip(t - w // 2, 0, N)
        hi = jnp.clip(t + w - w // 2, 0, N)
        mean = (cs[:, hi, sl] - cs[:, lo, sl]) / (hi - lo).astype(F32)[None, :, None]
        outs.append((mean.astype(z.dtype) - z[..., sl]) @ w_pool[gi])
    return jnp.concatenate(outs, axis=-1) * pool_scale


def merge_branches(attn, conv_in, pool_in, gate_in, lp):
    y_a = attn @ lp['w_attn_o']
    y_b = conformer_conv(conv_in, lp['conv_dw'], lp['conv_dw_b'], lp['conv_ln_g'], lp['conv_ln_b'], lp['w_conv_o'])
    y_c = multiscale_pool(pool_in, lp['w_pool'], lp['pool_scale']) @ lp['w_pool_o']
    g_a, g_b, g_c = jnp.split(jax.nn.sigmoid(gate_in), N_BRANCHES, axis=-1)
    return (g_a * y_a + g_b * y_b + g_c * y_c) @ lp['w_out']


def expert_choice_ffn(h, w_router, w_gate, w_up, w_down):
    B, N, _ = h.shape
    cap = (CAPACITY_FACTOR * N) // N_EXPERTS
    aff = jax.nn.softmax(jnp.einsum('bnd,de->bne', h, w_router).astype(F32), axis=-1)
    g, idx = lax.top_k(jnp.transpose(aff, (0, 2, 1)), cap)
    bidx = jnp.arange(B)[:, None, None]
    xe = h[bidx, idx]
    a = jnp.einsum('becd,edf->becf', xe, w_gate)
    u = jnp.einsum('becd,edf->becf', xe, w_up)
    ye = jnp.einsum('becf,efd->becd', jax.nn.silu(a) * u, w_down) * g[..., None].astype(h.dtype)
    return jnp.zeros_like(h).at[bidx, idx].add(ye)


def split_in(p):
    return jnp.split(p, IN_OFFSETS, axis=-1)


def setup_inputs(seed: int = 0) -> dict:
    key = jax.random.key(seed)
    ks = jax.random.split(key, 25)

    def nrm(k, shape, scale):
        return jax.random.normal(k, shape, jnp.float32) * scale

    return {
        'x': nrm(ks[0], (BATCH, SEQ, D_MODEL), 1.0),
        'c': nrm(ks[1], (BATCH, D_MODEL), 1.0),
        'ctx': nrm(ks[2], (BATCH, CTX_LEN, D_MODEL), 1.0),
        'c_ctx': nrm(ks[3], (D_MODEL,), 1.0),
        'norm1_g': 1.0 + nrm(ks[4], (DEPTH, D_MODEL), 0.02),
        'norm2_g': 1.0 + nrm(ks[5], (DEPTH, D_MODEL), 0.02),
        'w_mod': nrm(ks[6], (DEPTH, D_MODEL, 6 * D_MODEL), 0.02),
        'b_mod': nrm(ks[7], (DEPTH, 6 * D_MODEL), 0.02),
        'w_in': nrm(ks[8], (DEPTH, D_MODEL, IN_W), D_MODEL ** -0.5),
        'attn_sink': nrm(ks[9], (DEPTH, N_HEADS), 1.0),
        'w_attn_o': nrm(ks[10], (DEPTH, Q_W, D_MODEL), Q_W ** -0.5),
        'conv_dw': nrm(ks[11], (DEPTH, CONV_K, CONV_CH), CONV_K ** -0.5),
        'conv_dw_b': nrm(ks[12], (DEPTH, CONV_CH), 0.02),
        'conv_ln_g': 1.0 + nrm(ks[13], (DEPTH, CONV_CH), 0.02),
        'conv_ln_b': nrm(ks[14], (DEPTH, CONV_CH), 0.02),
        'w_conv_o': nrm(ks[15], (DEPTH, CONV_CH, D_MODEL), CONV_CH ** -0.5),
        'w_pool': nrm(ks[16], (DEPTH, len(POOL_WINDOWS), POOL_GROUP, POOL_GROUP), POOL_GROUP ** -0.5),
        'pool_scale': 1.0 + nrm(ks[17], (DEPTH, POOL_CH), 0.1),
        'w_pool_o': nrm(ks[18], (DEPTH, POOL_CH, D_MODEL), POOL_CH ** -0.5),
        'w_out': nrm(ks[19], (DEPTH, D_MODEL, D_MODEL), D_MODEL ** -0.5),
        'w_router': nrm(ks[20], (DEPTH, D_MODEL, N_EXPERTS), D_MODEL ** -0.5),
        'w_e_gate': nrm(ks[21], (DEPTH, N_EXPERTS, D_MODEL, D_EXPERT), D_MODEL ** -0.5),
        'w_e_up': nrm(ks[22], (DEPTH, N_EXPERTS, D_MODEL, D_EXPERT), D_MODEL ** -0.5),
        'w_e_down': nrm(ks[23], (DEPTH, N_EXPERTS, D_EXPERT, D_MODEL), D_EXPERT ** -0.5),
        'final_norm_g': 1.0 + nrm(ks[24], (D_MODEL,), 0.02),
    }


def reference(x, c, ctx, c_ctx, norm1_g, norm2_g, w_mod, b_mod, w_in, attn_sink, w_attn_o,
              conv_dw, conv_dw_b, conv_ln_g, conv_ln_b, w_conv_o, w_pool, pool_scale, w_pool_o,
              w_out, w_router, w_e_gate, w_e_up, w_e_down, final_norm_g):
    S = x.shape[1]
    tabs = rope_tables(S)
    for l in range(DEPTH):
        last = l == DEPTH - 1
        lp = {'w_attn_o': w_attn_o[l], 'conv_dw': conv_dw[l], 'conv_dw_b': conv_dw_b[l],
              'conv_ln_g': conv_ln_g[l], 'conv_ln_b': conv_ln_b[l], 'w_conv_o': w_conv_o[l],
              'w_pool': w_pool[l], 'pool_scale': pool_scale[l], 'w_pool_o': w_pool_o[l], 'w_out': w_out[l]}
        mod_x = (jax.nn.silu(c) @ w_mod[l] + b_mod[l])[:, None, :]
        sh1, sc1, g1, sh2, sc2, g2 = jnp.split(mod_x, 6, axis=-1)
        mod_c = jax.nn.silu(c_ctx) @ w_mod[l] + b_mod[l]
        csh1, csc1, cg1, csh2, csc2, cg2 = jnp.split(mod_c, 6, axis=-1)

        p_x = modulate(rmsnorm(x, norm1_g[l]), sh1, sc1) @ w_in[l]
        p_c = modulate(rmsnorm(ctx, norm1_g[l]), csh1, csc1) @ w_in[l]
        qx, kx, vx, conv_x, pool_x, gate_x = split_in(p_x)
        qc, kc, vc, conv_c, pool_c, gate_c = split_in(p_c)
        kc = heads(kc, N_KV_HEADS)
        vc = heads(vc, N_KV_HEADS)
        attn_x = window_attention(apply_axial_rope(heads(qx, N_HEADS), tabs),
                                  apply_axial_rope(heads(kx, N_KV_HEADS), tabs),
                                  heads(vx, N_KV_HEADS), kc, vc, attn_sink[l])
        x = x + g1 * merge_branches(attn_x, conv_x, pool_x, gate_x, lp)
        if not last:
            attn_c = context_attention(heads(qc, N_HEADS), kc, vc, attn_sink[l])
            ctx = ctx + cg1 * merge_branches(attn_c, conv_c, pool_c, gate_c, lp)

        x = x + g2 * expert_choice_ffn(modulate(rmsnorm(x, norm2_g[l]), sh2, sc2),
                                       w_router[l], w_e_gate[l], w_e_up[l], w_e_down[l])
        if not last:
            ctx = ctx + cg2 * expert_choice_ffn(modulate(rmsnorm(ctx, norm2_g[l]), csh2, csc2),
                                                w_router[l], w_e_gate[l], w_e_up[l], w_e_down[l])
    return rmsnorm(x, final_norm_g)
```

```python
import contextlib
import numpy as np
import ml_dtypes
import concourse.bass as bass
import concourse.mybir as mybir
from concourse.bass_utils import run_bass_kernel_spmd

F32 = mybir.dt.float32
BF16 = mybir.dt.bfloat16
I32 = mybir.dt.int32
U32 = mybir.dt.uint32
AF = mybir.ActivationFunctionType
ALU = mybir.AluOpType
AX = mybir.AxisListType

_DTSIZE = {F32: 4, BF16: 2, I32: 4, U32: 4}


class Res:
    def __init__(self, name, ap=None):
        self.name = name
        self.ap = ap
        self.w = {}
        self.r = {}
        self.parts = {}

    def part(self, k, col=0):
        key = (k, col // 2048)
        if key not in self.parts:
            self.parts[key] = Res("%s.%d.%d" % (self.name, key[0], key[1]), self.ap)
        return self.parts[key]


class _Eng:
    def __init__(self, name, sem):
        self.name = name
        self.sem = sem
        self.count = 0
        self.seen = {}
        self.items = []
        self.slots = []
        self.slot_i = 0


class Prog:
    NSLOT = 8
    SB_LIMIT = 212992

    def __init__(self):
        self.nc = bass.Bass("TRN2", target_bir_lowering=False)
        nc = self.nc
        self.stack = contextlib.ExitStack()
        self.sems = {}
        self.eng = {}
        for n in ("pe", "act", "dve", "pool", "sp"):
            s = self.stack.enter_context(nc.semaphore("s_" + n))
            self.sems[id(s)] = s
            self.eng[n] = _Eng(n, s)
        for n in ("sp", "act", "pool"):
            for i in range(self.NSLOT * {"sp": 4, "pool": 3, "act": 2}[n]):
                s = self.stack.enter_context(nc.semaphore("d_%s%d" % (n, i)))
                self.sems[id(s)] = s
                self.eng[n].slots.append([s, 0])
        self.psum = []
        for i in range(8):
            t = self.stack.enter_context(nc.psum_tensor("ps%d" % i, [128, 512], F32))
            self.psum.append(Res("ps%d" % i, t))
        self.sb_base = 16640
        self.sb_off = 16640
        self.nalloc = 0

    def sb(self, name, shape, dtype, persist=False):
        n = 1
        for s in shape[1:]:
            n *= s
        nbytes = (n * _DTSIZE[dtype] + 63) // 64 * 64
        off = self.sb_off
        assert off + nbytes <= getattr(self, "top_off", self.SB_LIMIT), ("SBUF overflow", name, off, nbytes)
        self.nalloc += 1
        h = self.nc.alloc_sbuf_tensor_at("%s_%d" % (name, self.nalloc), list(shape), dtype, offset=off)
        self.sb_off = off + nbytes
        if persist:
            assert self.sb_base == off
            self.sb_base = self.sb_off
        return Res(name, h)

    def sb_top(self, name, shape, dtype):
        n = 1
        for s_ in shape[1:]:
            n *= s_
        nbytes = (n * _DTSIZE[dtype] + 63) // 64 * 64
        self.top_off = getattr(self, "top_off", self.SB_LIMIT) - nbytes
        assert self.top_off >= self.sb_off, ("SBUF overflow (top)", name)
        self.nalloc += 1
        h = self.nc.alloc_sbuf_tensor_at("%s_%d" % (name, self.nalloc), list(shape), dtype, offset=self.top_off)
        return Res(name, h)

    def _collect(self, e, reads, writes):
        need = {}

        def add(d):
            for k, v in d.items():
                if need.get(k, 0) < v:
                    need[k] = v

        for r in reads:
            add(r.w)
        for w in writes:
            add(w.w)
            add(w.r)
        waits = []
        for k, v in need.items():
            if e.name == "pe" and k == id(e.sem):
                continue
            if e.seen.get(k, 0) >= v:
                continue
            e.seen[k] = v
            waits.append((self.sems[k], v))
        return waits

    def _commit(self, key, val, reads, writes):
        for r in reads:
            if r.r.get(key, 0) < val:
                r.r[key] = val
        for w in writes:
            if w.w.get(key, 0) < val:
                w.w[key] = val
            w.r = {}

    def op(self, en, fn, reads=(), writes=()):
        e = self.eng[en]
        waits = self._collect(e, reads, writes)
        e.count += 1
        e.items.append((waits, fn, (e.sem, 1)))
        self._commit(id(e.sem), e.count, reads, writes)

    def dma(self, en, out, in_, reads=(), writes=(), **kw):
        self._dma(en, lambda q: q.dma_start(out=out, in_=in_, **kw), reads, writes)

    def _dma(self, en, fn, reads, writes):
        e = self.eng[en]
        slot = e.slots[e.slot_i]
        e.slot_i = (e.slot_i + 1) % len(e.slots)
        waits = self._collect(e, reads, writes)
        k = id(slot[0])
        if slot[1] > 0 and e.seen.get(k, 0) < 16 * slot[1]:
            e.seen[k] = 16 * slot[1]
            waits.append((slot[0], 16 * slot[1]))
        slot[1] += 1
        e.items.append((waits, fn, (slot[0], 16)))
        self._commit(k, 16 * slot[1], reads, writes)

    def barrier(self):
        allv = {}
        for e in self.eng.values():
            allv[id(e.sem)] = e.count
            for s, c in e.slots:
                allv[id(s)] = 16 * c
        for e in self.eng.values():
            waits = []
            for k, v in allv.items():
                if k == id(e.sem) and e.name == "pe":
                    continue
                if v > 0 and e.seen.get(k, 0) < v:
                    e.seen[k] = v
                    waits.append((self.sems[k], v))
            if waits:
                e.items.append((waits, None, None))

    def phase_begin(self):
        self.barrier()
        self.hw = max(getattr(self, "hw", 0), self.sb_off)
        self.sb_off = self.sb_base
        self.top_off = self.SB_LIMIT

    def finish(self):
        self.barrier()
        nc = self.nc
        with nc.Block() as block:
            def mk(en):
                def run(q):
                    for waits, fn, inc in self.eng[en].items:
                        for wi, (s, v) in enumerate(waits):
                            q.wait_ge(s, v)
                            if wi + 1 < len(waits) or fn is None:
                                q.nop()
                        if fn is not None:
                            ins = fn(q)
                            ins.then_inc(inc[0], inc[1])
                return run
            block.tensor(mk("pe"))
            block.scalar(mk("act"))
            block.vector(mk("dve"))
            block.gpsimd(mk("pool"))
            block.sync(mk("sp"))
        self.stack.close()


D = 1024
S = 4096
L = 256
NT = S + L
INW = 5376
NEXP = 16
EPS = 1e-6
OFF_Q, OFF_K, OFF_V, OFF_CA, OFF_CG, OFF_P, OFF_G = 0, 512, 640, 768, 1280, 1792, 2304
M_SH1, M_GS1, M_G1, M_SH2, M_GS2, M_G2 = range(6)


def tiles_for(layer, with_ctx_always=False):
    ts = [(i * 512, 512, 0) for i in range(8)]
    if layer == 0 or with_ctx_always:
        ts.append((S, L, 1))
    return ts


class K:
    def __init__(self, debug=False):
        self.P = Prog()
        self.debug = debug
        P = self.P
        nc = P.nc
        self.nc = nc

        def din(name, shape, dt=F32):
            return nc.dram_tensor(name, list(shape), dt, kind="ExternalInput").ap()

        def dscr(name, shape, dt):
            kind = "ExternalOutput"
            return nc.dram_tensor(name, list(shape), dt, kind=kind).ap()

        self.i = {}
        I = self.i
        I["x"] = din("x", [S, D]); I["ctx"] = din("ctx", [L, D])
        I["cvec"] = din("cvec", [128, 8, 2])
        I["ng"] = din("ng", [2, 2, 128, D]); I["fng"] = din("fng", [128, D])
        I["w_mod"] = din("w_mod", [2, D, 6 * D]); I["bmod"] = din("bmod", [2, 128, 6 * D])
        I["w_in"] = din("w_in", [2, D, INW])
        I["sink"] = din("sink", [2, 128, 8])
        I["w_attn_o"] = din("w_attn_o", [2, 512, D])
        I["dwt"] = din("dwt", [2, 128, 4, 31]); I["cvp"] = din("cvp", [2, 128, 3, 4])
        I["w_conv_o"] = din("w_conv_o", [2, 512, D])
        I["w_pool"] = din("w_pool", [2, 4, 128, 128]); I["psc"] = din("psc", [2, 128, 4])
        I["w_pool_o"] = din("w_pool_o", [2, 512, D]); I["w_out"] = din("w_out", [2, D, D])
        I["w_router"] = din("w_router", [2, D, NEXP])
        I["w_e_gate"] = din("w_e_gate", [2, NEXP, D, D]); I["w_e_up"] = din("w_e_up", [2, NEXP, D, D])
        I["w_e_down"] = din("w_e_down", [2, NEXP, D, D])
        I["cb16"] = din("cb16", [128, 7, 128], BF16)
        I["mk4"] = din("mk4", [128, 2, 512], BF16)
        I["identf"] = din("identf", [128, 128]); I["iota"] = din("iota", [128, 512])
        I["tokv"] = din("tokv", [128, 34, 2], BF16)
        I["rope"] = din("rope", [4, 64, NT])
        I["invc"] = din("invc", [128, 4, NT])
        self.out = nc.dram_tensor("out", [S, D], F32, kind="ExternalOutput").ap()
        self.XR = dscr("XR", [NT, D], F32)
        self.PT = dscr("PT", [INW, NT], BF16)
        self.AT = dscr("AT", [512, NT], BF16)
        self.ST = dscr("ST", [512, NT], BF16)
        self.DT = dscr("DT", [512, NT], BF16)
        self.H2 = dscr("H2", [NT, D], BF16)
        self.MODD = dscr("MODD", [2, 6, 128, D], F32)
        nb = NT // 128
        self.rXR = [Res("XR%d" % i) for i in range(nb)]
        self.rPT = [Res("PT%d" % i) for i in range(nb)]
        self.rAT = [Res("AT%d" % i) for i in range(nb)]
        self.rST = [Res("ST%d" % i) for i in range(nb)]
        self.rDT = [Res("DT%d" % i) for i in range(nb)]
        self.rH2 = Res("H2")
        self.rMODD = Res("MODD")
        self.rXRS = Res("XRS")

    def xrow_src(self, l, r0):
        if l == 0:
            if r0 < S:
                return self.i["x"][r0:r0 + 128, :]
            return self.i["ctx"][r0 - S:r0 - S + 128, :]
        return self.XR[r0:r0 + 128, :]

    def blk(self, rl, t0, t1):
        t0 = max(t0, 0); t1 = min(t1, NT)
        return rl[t0 // 128:(t1 + 127) // 128]

    def setup(self):
        P, I = self.P, self.i
        self.cb = P.sb("cb16", [128, 7, 128], BF16, persist=True)
        self.mk4 = P.sb("mk4", [128, 2, 512], BF16, persist=True)
        self.identf = P.sb("identf", [128, 128], F32, persist=True)
        self.iota = P.sb("iota", [128, 512], F32, persist=True)
        self.tokv = P.sb("tokv", [128, 34, 2], BF16, persist=True)
        self.IDX = P.sb("IDX", [128, 80], I32, persist=True)
        self.GV = P.sb("GV", [128, 80], F32, persist=True)
        self.onesf = P.sb("onesf", [128, 128], F32, persist=True)
        P.dma("sp", self.cb.ap[:], I["cb16"][:, :, :], writes=[self.cb])
        P.dma("sp", self.mk4.ap[:], I["mk4"][:, :, :], writes=[self.mk4])
        P.dma("sp", self.identf.ap[:], I["identf"][:, :], writes=[self.identf])
        P.dma("sp", self.iota.ap[:], I["iota"][:, :], writes=[self.iota])
        P.dma("sp", self.tokv.ap[:], I["tokv"][:, :, :], writes=[self.tokv])
        P.op("dve", lambda e: e.memset(self.onesf.ap[:], 1.0), writes=[self.onesf])
        P.op("dve", lambda e: e.memset(self.IDX.ap[:], 0), writes=[self.IDX])
        P.op("dve", lambda e: e.memset(self.GV.ap[:], 0.0), writes=[self.GV])

    def ident(self):
        return self.cb.ap[:, 0, :]

    def ones(self):
        return self.cb.ap[:, 1, :]

    def load_cast_rows(self, dst, src_rows_ap, ncols, writes, kchunks):
        P = self.P
        for c0 in range(0, ncols, 2048):
            c1 = min(c0 + 2048, ncols)
            for k in range(kchunks):
                P.dma("pool", dst.ap[:, k, c0:c1], src_rows_ap[k * 128:(k + 1) * 128, c0:c1], writes=[dst.part(k, c0)])

    def phase_mod(self, l):
        P, I = self.P, self.i
        P.phase_begin()
        cv = P.sb("cv", [128, 8, 2], F32)
        sc = P.sb("sc", [128, 8, 2], F32)
        screp = P.sb("screp", [128, 2, 8, 128], BF16)
        wms = [P.sb("wm%d" % i, [128, 8, 3072], BF16) for i in range(2)]
        bms = [P.sb("bm%d" % i, [128, 3072], F32) for i in range(2)]
        ngt = P.sb("ngt", [128, 2, D], F32)
        mos = [P.sb("mo%d" % i, [128, 2, 3072], F32) for i in range(2)]
        P.dma("sp", cv.ap[:], I["cvec"][:, :, :], writes=[cv])
        P.dma("sp", ngt.ap[:], I["ng"][l].rearrange("n p d -> p n d"), writes=[ngt])
        for half in range(2):
            c0 = half * 3072
            self.load_cast_rows(wms[half], I["w_mod"][l][:, c0:c0 + 3072], 3072, None, 8)
            P.dma("sp", bms[half].ap[:], I["bmod"][l][:, c0:c0 + 3072], writes=[bms[half]])
        P.op("act", lambda e: e.activation(out=sc.ap[:], in_=cv.ap[:], func=AF.Silu), reads=[cv], writes=[sc])
        for s in range(2):
            for k in range(8):
                P.op("dve", lambda e, s=s, k=k: e.tensor_scalar(
                    out=screp.ap[:, s, k, :], in0=self.onesf.ap[:], scalar1=sc.ap[:, k, s:s + 1], scalar2=None,
                    op0=ALU.mult), reads=[sc, self.onesf], writes=[screp])
        npz = 0
        for half in range(2):
            wm = wms[half]; bm = bms[half]; mo = mos[half]
            for s in range(2):
                for j in range(6):
                    ps = P.psum[npz % 8]; npz += 1
                    for k in range(8):
                        P.op("pe", lambda e, ps=ps, s=s, k=k, j=j, wm=wm: e.matmul(
                            ps.ap[:, :], lhsT=screp.ap[:, s, k, :], rhs=wm.ap[:, k, j * 512:(j + 1) * 512],
                            start=(k == 0), stop=(k == 7)), reads=[screp, wm.part(k, j * 512)], writes=[ps])
                    P.op("dve", lambda e, ps=ps, s=s, j=j, bm=bm, mo=mo: e.tensor_tensor(
                        out=mo.ap[:, s, j * 512:(j + 1) * 512], in0=ps.ap[:, :], in1=bm.ap[:, j * 512:(j + 1) * 512],
                        op=ALU.add), reads=[ps, bm], writes=[mo])
            for s in range(2):
                P.op("dve", lambda e, s=s, half=half, mo=mo: e.scalar_tensor_tensor(
                    out=mo.ap[:, s, 1024:2048], in0=mo.ap[:, s, 1024:2048], scalar=1.0, in1=ngt.ap[:, half, :],
                    op0=ALU.add, op1=ALU.mult), reads=[mo, ngt], writes=[mo])
                P.dma("sp", self.MODD[s, half * 3:(half + 1) * 3].rearrange("c p d -> p c d"),
                      mo.ap[:, s, :].rearrange("p (c d) -> p c d", c=3), reads=[mo], writes=[self.rMODD])

    def load_mod(self, name, s, chunk):
        P = self.P
        t = P.sb(name, [128, D], F32)
        P.dma("sp", t.ap[:], self.MODD[s, chunk], reads=[self.rMODD], writes=[t])
        return t

    def norm_mod(self, xt, gs, sh, hb, ss, tmp):
        P = self.P
        P.op("act", lambda e: e.activation(out=tmp.ap[:], in_=xt.ap[:], func=AF.Square, accum_out=ss.ap[:, 0:1]),
             reads=[xt], writes=[tmp, ss])
        P.op("dve", lambda e: e.tensor_scalar(out=ss.ap[:, 1:2], in0=ss.ap[:, 0:1], scalar1=1.0 / D, scalar2=EPS,
                                              op0=ALU.mult, op1=ALU.add), reads=[ss], writes=[ss])
        P.op("act", lambda e: e.activation(out=ss.ap[:, 1:2], in_=ss.ap[:, 1:2], func=AF.Ln), reads=[ss], writes=[ss])
        P.op("act", lambda e: e.activation(out=ss.ap[:, 1:2], in_=ss.ap[:, 1:2], func=AF.Exp, scale=-0.5), reads=[ss], writes=[ss])
        if sh is None:
            P.op("dve", lambda e: e.scalar_tensor_tensor(out=hb.ap[:], in0=xt.ap[:], scalar=ss.ap[:, 1:2], in1=gs.ap[:],
                                                         op0=ALU.mult, op1=ALU.mult), reads=[xt, ss, gs], writes=[hb])
            return
        P.op("dve", lambda e: e.scalar_tensor_tensor(out=xt.ap[:], in0=xt.ap[:], scalar=ss.ap[:, 1:2], in1=gs.ap[:],
                                                     op0=ALU.mult, op1=ALU.mult), reads=[xt, ss, gs], writes=[xt])
        P.op("dve", lambda e: e.tensor_tensor(out=hb.ap[:], in0=xt.ap[:], in1=sh.ap[:], op=ALU.add),
             reads=[xt, sh], writes=[hb])

    def transpose8(self, hb, ps, dst_ap, dst_res, nrows=128, eng="act"):
        P = self.P
        pv = ps.ap[:, :].bitcast(BF16)
        for k in range(8):
            P.op("pe", lambda e, k=k: e.transpose(out=pv[:, k * 128:k * 128 + nrows], in_=hb.ap[0:nrows, k * 128:(k + 1) * 128],
                                                  identity=self.cb.ap[0:nrows, 0, 0:nrows]),
                 reads=[hb, self.cb], writes=[ps])
        src = pv.rearrange("p (k t) -> p k t", k=8)[:, :, 0:nrows]
        if eng == "act":
            P.op("act", lambda e: e.copy(out=dst_ap, in_=src), reads=[ps], writes=[dst_res])
        else:
            P.op("dve", lambda e: e.tensor_copy(out=dst_ap, in_=src), reads=[ps], writes=[dst_res])

    def phase_inproj(self, l):
        P, I = self.P, self.i
        P.phase_begin()
        win = P.sb("win", [128, 8, INW], BF16)
        gsx = [self.load_mod("gs%d" % s, s, M_GS1) for s in range(2)]
        shx = [self.load_mod("sh%d" % s, s, M_SH1) for s in range(2)]
        self.load_cast_rows(win, I["w_in"][l], INW, [win], 8)
        xts = [P.sb("xt%d" % i, [128, D], F32) for i in range(2)]
        hbs = [P.sb("hb%d" % i, [128, D], BF16) for i in range(2)]
        junk = P.sb("junk", [128, D], BF16)
        sss = [P.sb("ss%d" % i, [128, 2], F32) for i in range(2)]
        hTs = [P.sb("hT%d" % i, [128, 8, 512], BF16) for i in range(2)]
        obs = [P.sb("ob%d" % i, [128, 7, 512], BF16) for i in range(2)]
        st = {"nsub": 0, "ngrp": 0, "npj": 0}
        tiles = tiles_for(l, True)

        def prep(ti):
            (t0, TT, s) = tiles[ti]
            hT = hTs[ti % 2]
            for sub in range(TT // 128):
                n = st["nsub"]; st["nsub"] += 1
                xt = xts[n % 2]; hb = hbs[n % 2]; ss = sss[n % 2]
                r0 = t0 + sub * 128
                P.dma("sp", xt.ap[:], self.xrow_src(l, r0), reads=self.blk(self.rXR, r0, r0 + 128), writes=[xt])
                self.norm_mod(xt, gsx[s], shx[s], hb, ss, junk)
                self.transpose8(hb, P.psum[n % 2], hT.ap[:, :, sub * 128:(sub + 1) * 128], hT)

        def proj(ti, groups):
            (t0, TT, s) = tiles[ti]
            hT = hTs[ti % 2]
            for grp in groups:
                ob = obs[st["ngrp"] % 2]
                st["ngrp"] += 1
                for ci, c in enumerate(grp):
                    ps = P.psum[2 + st["npj"] % 6]
                    st["npj"] += 1
                    for k in range(8):
                        P.op("pe", lambda e, ps=ps, k=k, c=c, TT=TT, hT=hT: e.matmul(
                            ps.ap[:, 0:TT], lhsT=win.ap[:, k, c * 128:(c + 1) * 128], rhs=hT.ap[:, k, 0:TT],
                            start=(k == 0), stop=(k == 7)), reads=[win.part(k, c * 128), hT], writes=[ps])
                    sig = (c >= 18) or (10 <= c < 14)
                    if sig:
                        P.op("act", lambda e, ps=ps, ci=ci, TT=TT, ob=ob: e.activation(
                            out=ob.ap[:, ci, 0:TT], in_=ps.ap[:, 0:TT], func=AF.Sigmoid), reads=[ps], writes=[ob])
                    else:
                        P.op("dve", lambda e, ps=ps, ci=ci, TT=TT, ob=ob: e.tensor_copy(
                            out=ob.ap[:, ci, 0:TT], in_=ps.ap[:, 0:TT]), reads=[ps], writes=[ob])
                c0, n = grp[0], len(grp)
                P.dma("pool", self.PT[c0 * 128:(c0 + n) * 128, t0:t0 + TT].rearrange("(c p) t -> p c t", p=128),
                      ob.ap[:, 0:n, 0:TT], reads=[ob], writes=self.blk(self.rPT, t0, t0 + TT))

        prep(0)
        for ti, (t0, TT, s) in enumerate(tiles):
            if s == 1 and l == 1:
                groups = [[4, 5]]
            else:
                groups = [list(range(g * 7, g * 7 + 7)) for g in range(6)]
            h = len(groups) // 2
            proj(ti, groups[:h])
            if ti + 1 < len(tiles):
                prep(ti + 1)
            proj(ti, groups[h:])

    def rope(self, dst_ap, dst_res, raw_ap, swp_ap, cos_ap, sin_ap, t1, t2, reads, npart=64):
        P = self.P
        P.op("dve", lambda e: e.tensor_tensor(out=t1.ap[0:npart, 0:cos_ap.shape[-1]], in0=raw_ap, in1=cos_ap, op=ALU.mult),
             reads=reads, writes=[t1])
        P.op("dve", lambda e: e.tensor_tensor(out=t2.ap[0:npart, 0:cos_ap.shape[-1]], in0=swp_ap, in1=sin_ap, op=ALU.mult),
             reads=reads, writes=[t2])
        P.op("dve", lambda e: e.tensor_tensor(out=dst_ap, in0=t1.ap[0:npart, 0:cos_ap.shape[-1]],
                                              in1=t2.ap[0:npart, 0:cos_ap.shape[-1]], op=ALU.add),
             reads=[t1, t2], writes=[dst_res])

    def phase_attn(self, l):
        P, I = self.P, self.i
        PERM = [1, 0, 3, 2]
        kT = P.sb("kT", [64, 2, NT], BF16)
        vtok = P.sb("vtok", [128, 34, 128], BF16)
        snk = P.sb("snk", [128, 8], F32)
        esk = P.sb("esk", [64, 2, 512], F32)
        t1 = P.sb("t1", [64, 512], F32)
        t2 = P.sb("t2", [64, 512], F32)
        cosk = P.sb("cosk", [64, 512], F32); sink_ = P.sb("sink_", [64, 512], F32)
        kraws = [P.sb("kraw%d" % i, [64, 512], BF16) for i in range(2)]
        kswps = [P.sb("kswp%d" % i, [64, 512], BF16) for i in range(2)]
        vT = P.sb("vT", [128, 512], BF16)
        P.dma("sp", snk.ap[:], I["sink"][l], writes=[snk])
        P.op("act", lambda e: e.activation(out=snk.ap[:], in_=snk.ap[:], func=AF.Exp), reads=[snk], writes=[snk])
        for g in range(2):
            for h in range(4):
                P.op("dve", lambda e, g=g, h=h: e.tensor_scalar(
                    out=esk.ap[:, g, h * 128:(h + 1) * 128], in0=self.onesf.ap[0:64, :], scalar1=snk.ap[0:64, g * 4 + h:g * 4 + h + 1],
                    scalar2=None, op0=ALU.mult), reads=[snk, self.onesf], writes=[esk])
        nv = 0
        attn_mark = P.sb_off
        for (t0, TT, s) in tiles_for(l, True):
            rd = self.blk(self.rPT, t0, t0 + TT)
            P.dma("sp", cosk.ap[:, 0:TT], I["rope"][0, :, t0:t0 + TT], writes=[cosk])
            P.dma("sp", sink_.ap[:, 0:TT], I["rope"][1, :, t0:t0 + TT], writes=[sink_])
            for g in range(2):
                kraw = kraws[g]; kswp = kswps[g]
                base = OFF_K + g * 64
                P.dma("sp", kraw.ap[:, 0:TT], self.PT[base:base + 64, t0:t0 + TT], reads=rd, writes=[kraw])
                for j in range(4):
                    P.dma("sp", kswp.ap[j * 16:(j + 1) * 16, 0:TT],
                          self.PT[base + PERM[j] * 16:base + PERM[j] * 16 + 16, t0:t0 + TT], reads=rd, writes=[kswp])
                self.rope(kT.ap[:, g, t0:t0 + TT], kT, kraw.ap[:, 0:TT], kswp.ap[:, 0:TT], cosk.ap[:, 0:TT], sink_.ap[:, 0:TT],
                          t1, t2, [kraw, kswp, cosk, sink_])
            P.dma("sp", vT.ap[:, 0:TT], self.PT[OFF_V:OFF_V + 128, t0:t0 + TT], reads=rd, writes=[vT])
            for sub in range(TT // 128):
                ps = P.psum[nv % 2]
                nv += 1
                pv = ps.ap[:, :].bitcast(BF16)
                P.op("pe", lambda e, pv=pv, sub=sub: e.transpose(out=pv[:, 0:128], in_=vT.ap[:, sub * 128:(sub + 1) * 128],
                                                                 identity=self.ident()), reads=[vT, self.cb], writes=[ps])
                P.op("act", lambda e, pv=pv, b=(t0 // 128 + sub): e.copy(out=vtok.ap[:, b, :], in_=pv[:, 0:128]),
                     reads=[ps], writes=[vtok])
            yield "kv"
        yield "kvdone"
        P.barrier()
        P.hw = max(getattr(P, "hw", 0), P.sb_off)
        P.sb_off = attn_mark
        self._ow = self.outp_weights(l, load=True)
        self._ow_top = P.top_off
        qraws = [P.sb("qraw%d" % i, [64, 4, 512], BF16) for i in range(2)]
        qswps = [P.sb("qswp%d" % i, [64, 4, 512], BF16) for i in range(2)]
        qrs = [P.sb("qr%d" % i, [64, 4, 512], BF16) for i in range(2)]
        cosqs = [P.sb("cosq%d" % i, [64, 512], F32) for i in range(2)]
        sinqs = [P.sb("sinq%d" % i, [64, 512], F32) for i in range(2)]
        t1b = P.sb("t1b", [64, 512], F32); t2b = P.sb("t2b", [64, 512], F32)
        ets = [P.sb("et%d" % i, [128, 512], BF16) for i in range(4)]
        dens = [P.sb("den%d" % i, [64, 512], F32) for i in range(2)]
        ats = [P.sb("at%d" % i, [64, 512], BF16) for i in range(2)]
        LA = 3
        groups = [(ti, t0, TT, s_, g) for ti, (t0, TT, s_) in enumerate(tiles_for(l)) for g in range(2)]

        def prologue(gi):
            (ti, t0, TT, s_, g) = groups[gi]
            rd = self.blk(self.rPT, t0, t0 + TT)
            cosq = cosqs[ti % 2]; sinq = sinqs[ti % 2]
            qraw = qraws[gi % 2]; qswp = qswps[gi % 2]; qr = qrs[gi % 2]
            if g == 0:
                P.dma("sp", cosq.ap[:, 0:TT], I["rope"][2, :, t0:t0 + TT], writes=[cosq])
                P.dma("sp", sinq.ap[:, 0:TT], I["rope"][3, :, t0:t0 + TT], writes=[sinq])
            qrows = self.PT[g * 256:(g + 1) * 256, t0:t0 + TT]
            P.dma("sp", qraw.ap[:, :, 0:TT], qrows.rearrange("(h d) t -> d h t", d=64), reads=rd, writes=[qraw])
            q4 = qrows.rearrange("(h j d) t -> j d h t", j=4, d=16)
            for j in range(4):
                P.dma("sp", qswp.ap[j * 16:(j + 1) * 16, :, 0:TT], q4[PERM[j]], reads=rd, writes=[qswp])
            for h in range(4):
                self.rope(qr.ap[:, h, 0:TT], qr, qraw.ap[:, h, 0:TT], qswp.ap[:, h, 0:TT], cosq.ap[:, 0:TT], sinq.ap[:, 0:TT],
                          t1b, t2b, [qraw, qswp, cosq, sinq])

        cnt = {"S": 0, "Q": 0}

        def emit_S(step):
            (qr, g, qb, kb, mk, psN, psD, first, last, unit) = step["a"]
            psS = P.psum[cnt["S"] % 4]; et = ets[cnt["S"] % 4]; cnt["S"] += 1
            step["et"] = et
            P.op("pe", lambda e: e.matmul(psS.ap[:, :], lhsT=kT.ap[:, g, kb * 128:(kb + 1) * 128], rhs=qr.ap[:, :, qb * 128:(qb + 1) * 128],
                                          start=True, stop=True), reads=[kT, qr], writes=[psS])
            P.op("act", lambda e: e.activation(out=et.ap[:], in_=psS.ap[:, :], func=AF.Exp), reads=[psS], writes=[et])
            if mk is not None:
                P.op("dve", lambda e: e.tensor_tensor(out=et.ap[:], in0=et.ap[:], in1=self.mk4.ap[:, mk, :], op=ALU.mult),
                     reads=[et, self.mk4], writes=[et])

        def emit_PV(step):
            (qr, g, qb, kb, mk, psN, psD, first, last, unit) = step["a"]
            et = step["et"]
            P.op("pe", lambda e: e.matmul(psN.ap[0:64, :], lhsT=vtok.ap[:, kb, g * 64:(g + 1) * 64], rhs=et.ap[:], start=first, stop=last),
                 reads=[vtok, et], writes=[psN])
            P.op("pe", lambda e: e.matmul(psD.ap[0:64, :], lhsT=self.cb.ap[:, 1, 0:64], rhs=et.ap[:], start=first, stop=last),
                 reads=[self.cb, et], writes=[psD])
            if last:
                (nq, c0) = unit
                at = ats[nq % 2]; den = dens[nq % 2]
                P.op("dve", lambda e: e.tensor_tensor(out=den.ap[:], in0=psD.ap[0:64, :], in1=esk.ap[:, g, :], op=ALU.add),
                     reads=[psD, esk], writes=[den])
                P.op("act", lambda e: e.activation(out=den.ap[:], in_=den.ap[:], func=AF.Ln), reads=[den], writes=[den])
                P.op("act", lambda e: e.activation(out=den.ap[:], in_=den.ap[:], func=AF.Exp, scale=-1.0), reads=[den], writes=[den])
                P.op("dve", lambda e: e.tensor_tensor(out=at.ap[:], in0=psN.ap[0:64, :], in1=den.ap[:], op=ALU.mult),
                     reads=[psN, den], writes=[at])
                P.dma("pool", self.AT[g * 256:(g + 1) * 256, c0:c0 + 128].rearrange("(h d) t -> d h t", d=64),
                      at.ap[:].rearrange("d (h t) -> d h t", h=4), reads=[at], writes=self.blk(self.rAT, c0, c0 + 128))

        prologue(0)
        for gi, (ti, t0, TT, s_, g) in enumerate(groups):
            qr = qrs[gi % 2]
            steps = []
            for qb in range(TT // 128):
                n = t0 // 128 + qb
                if s_ == 0:
                    kbs = []
                    if n > 0:
                        kbs.append((n - 1, 0))
                    kbs.append((n, None))
                    if n < 31:
                        kbs.append((n + 1, 1))
                    kbs += [(32, None), (33, None)]
                else:
                    kbs = [(32, None), (33, None)]
                nq = cnt["Q"]; cnt["Q"] += 1
                psN = P.psum[4 + nq % 2]; psD = P.psum[6 + nq % 2]
                for i, (kb, mk) in enumerate(kbs):
                    steps.append({"a": (qr, g, qb, kb, mk, psN, psD, i == 0, i == len(kbs) - 1, (nq, t0 + qb * 128))})
            mid = len(steps) // 2
            for j in range(len(steps) + LA):
                if j < len(steps):
                    emit_S(steps[j])
                if j == mid and gi + 1 < len(groups):
                    prologue(gi + 1)
                if j >= LA:
                    emit_PV(steps[j - LA])
            if g == 1:
                yield "attn"

    def phase_conv(self, l):
        P, I = self.P, self.i
        dwt = P.sb("dwt", [128, 4, 31], F32)
        cvp = P.sb("cvp", [128, 3, 4], F32)
        diag = P.sb("diag", [128, 4, 31, 128], BF16)
        P.dma("sp", dwt.ap[:], I["dwt"][l], writes=[dwt])
        P.dma("sp", cvp.ap[:], I["cvp"][l], writes=[cvp])
        identf32 = self.identf
        for c in range(4):
            P.op("dve", lambda e, o=diag.ap[:, c, :, :], i0_=identf32.ap[:, :].unsqueeze(1).broadcast_to([128, 31, 128]),
                 i1_=dwt.ap[:, c, :].unsqueeze(2).broadcast_to([128, 31, 128]): e.tensor_tensor(out=o, in0=i0_, in1=i1_, op=ALU.mult),
                 reads=[dwt, identf32], writes=[diag])
        W = 512 + 30
        uas = [P.sb("ua%d" % i, [128, W], BF16) for i in range(4)]
        ugs = [P.sb("ug%d" % i, [128, W], BF16) for i in range(4)]
        us = [P.sb("u%d" % i, [128, W], BF16) for i in range(4)]
        cvs = [P.sb("cvv%d" % i, [128, 4, 512], F32) for i in range(2)]
        vbs = [P.sb("vb%d" % i, [128, 4, 512], BF16) for i in range(1)] * 2
        vsqs = [P.sb("vsq%d" % i, [128, 4, 512], BF16) for i in range(1)] * 2
        mean = P.sb("mean", [128, 512], F32)
        rstd = P.sb("rstd", [128, 512], F32)
        yts = [P.sb("yt%d" % i, [128, 512], F32) for i in range(4)]
        sts = [P.sb("st%d" % i, [128, 4, 512], BF16) for i in range(2)]
        nu = 0; nt = 0
        for (t0, TT, s) in tiles_for(l):
            cv = cvs[nt % 2]; vb = vbs[nt % 2]; vsq = vsqs[nt % 2]
            s0, N = (0, S) if s == 0 else (S, L)
            lo = max(t0 - 15, s0); hi = min(t0 + TT + 15, s0 + N)
            j0 = lo - (t0 - 15); j1 = j0 + (hi - lo)
            WW = TT + 30
            rd = self.blk(self.rPT, lo, hi)
            for c in range(4):
                ua, ug, u = uas[nu % 4], ugs[nu % 4], us[nu % 4]
                nu += 1
                if j0 > 0 or j1 < WW:
                    P.op("pool", lambda e, cv=cv, vb=vb, vsq=vsq, ua=ua: e.memset(ua.ap[:], 0.0), writes=[ua])
                    P.op("pool", lambda e, cv=cv, vb=vb, vsq=vsq, ug=ug: e.memset(ug.ap[:], 0.0), writes=[ug])
                ra = OFF_CA + c * 128; rg = OFF_CG + c * 128
                P.dma("sp", ua.ap[:, j0:j1], self.PT[ra:ra + 128, lo:hi], reads=rd, writes=[ua])
                P.dma("sp", ug.ap[:, j0:j1], self.PT[rg:rg + 128, lo:hi], reads=rd, writes=[ug])
                P.op("pool", lambda e, cv=cv, vb=vb, vsq=vsq, ua=ua, ug=ug, u=u, WW=WW: e.tensor_tensor(out=u.ap[:, 0:WW], in0=ua.ap[:, 0:WW], in1=ug.ap[:, 0:WW],
                                                                         op=ALU.mult), reads=[ua, ug], writes=[u])
                ps = P.psum[2 + c % 2]
                for k in range(31):
                    P.op("pe", lambda e, cv=cv, vb=vb, vsq=vsq, ps=ps, c=c, k=k, u=u, TT=TT: e.matmul(
                        ps.ap[:, 0:TT], lhsT=diag.ap[:, c, k, :], rhs=u.ap[:, k:k + TT], start=(k == 0), stop=(k == 30)),
                        reads=[diag, u], writes=[ps])
                P.op("act", lambda e, cv=cv, vb=vb, vsq=vsq, ps=ps, c=c, TT=TT: e.activation(out=cv.ap[:, c, 0:TT], in_=ps.ap[:, 0:TT], func=AF.Identity,
                                                                     bias=cvp.ap[:, 0, c:c + 1]), reads=[ps, cvp], writes=[cv])
                P.op("act", lambda e, cv=cv, vb=vb, vsq=vsq, ps=ps, c=c, TT=TT: e.activation(out=vsq.ap[:, c, 0:TT], in_=ps.ap[:, 0:TT], func=AF.Square,
                                                                     bias=cvp.ap[:, 0, c:c + 1]), reads=[ps, cvp], writes=[vsq])
                P.op("act", lambda e, cv=cv, vb=vb, vsq=vsq, ps=ps, c=c, TT=TT: e.activation(out=vb.ap[:, c, 0:TT], in_=ps.ap[:, 0:TT], func=AF.Identity,
                                                                     bias=cvp.ap[:, 0, c:c + 1]), reads=[ps, cvp], writes=[vb])
            psM = P.psum[4]; psQ = P.psum[5]
            for c in range(4):
                P.op("pe", lambda e, cv=cv, vb=vb, vsq=vsq, c=c, TT=TT: e.matmul(psM.ap[:, 0:TT], lhsT=self.cb.ap[:, 3, :], rhs=vb.ap[:, c, 0:TT],
                                                          start=(c == 0), stop=(c == 3)), reads=[self.cb, vb], writes=[psM])
            for c in range(4):
                P.op("pe", lambda e, cv=cv, vb=vb, vsq=vsq, c=c, TT=TT: e.matmul(psQ.ap[:, 0:TT], lhsT=self.cb.ap[:, 3, :], rhs=vsq.ap[:, c, 0:TT],
                                                          start=(c == 0), stop=(c == 3)), reads=[self.cb, vsq], writes=[psQ])
            yield "convA"
            P.op("act", lambda e, cv=cv, vb=vb, vsq=vsq, TT=TT: e.copy(out=mean.ap[:, 0:TT], in_=psM.ap[:, 0:TT]), reads=[psM], writes=[mean])
            P.op("dve", lambda e, cv=cv, vb=vb, vsq=vsq, TT=TT: e.tensor_tensor(out=rstd.ap[:, 0:TT], in0=mean.ap[:, 0:TT], in1=mean.ap[:, 0:TT], op=ALU.mult),
                 reads=[mean], writes=[rstd])
            P.op("dve", lambda e, cv=cv, vb=vb, vsq=vsq, TT=TT: e.tensor_tensor(out=rstd.ap[:, 0:TT], in0=psQ.ap[:, 0:TT], in1=rstd.ap[:, 0:TT], op=ALU.subtract),
                 reads=[psQ, rstd], writes=[rstd])
            P.op("dve", lambda e, cv=cv, vb=vb, vsq=vsq, TT=TT: e.tensor_scalar(out=rstd.ap[:, 0:TT], in0=rstd.ap[:, 0:TT], scalar1=0.0, scalar2=EPS,
                                                         op0=ALU.max, op1=ALU.add), reads=[rstd], writes=[rstd])
            P.op("act", lambda e, cv=cv, vb=vb, vsq=vsq, TT=TT: e.activation(out=rstd.ap[:, 0:TT], in_=rstd.ap[:, 0:TT], func=AF.Sqrt), reads=[rstd], writes=[rstd])
            P.op("dve", lambda e, cv=cv, vb=vb, vsq=vsq, TT=TT: e.reciprocal(out=rstd.ap[:, 0:TT], in_=rstd.ap[:, 0:TT]), reads=[rstd], writes=[rstd])
            st = sts[nt % 2]; nt += 1
            for c in range(4):
                yt = yts[c]
                P.op("dve", lambda e, cv=cv, vb=vb, vsq=vsq, c=c, TT=TT, yt=yt: e.tensor_tensor(out=yt.ap[:, 0:TT], in0=cv.ap[:, c, 0:TT], in1=mean.ap[:, 0:TT], op=ALU.subtract),
                     reads=[cv, mean], writes=[yt])
                P.op("dve", lambda e, cv=cv, vb=vb, vsq=vsq, TT=TT, yt=yt: e.tensor_tensor(out=yt.ap[:, 0:TT], in0=yt.ap[:, 0:TT], in1=rstd.ap[:, 0:TT], op=ALU.mult),
                     reads=[yt, rstd], writes=[yt])
                P.op("act", lambda e, cv=cv, vb=vb, vsq=vsq, c=c, TT=TT, st=st, yt=yt: e.activation(out=st.ap[:, c, 0:TT], in_=yt.ap[:, 0:TT], func=AF.Silu,
                                                                     scale=cvp.ap[:, 1, c:c + 1], bias=cvp.ap[:, 2, c:c + 1]),
                     reads=[yt, cvp], writes=[st])
            P.dma("act", self.ST[:, t0:t0 + TT].rearrange("(c p) t -> p c t", p=128), st.ap[:, :, 0:TT], reads=[st],
                  writes=self.blk(self.rST, t0, t0 + TT))
            yield "conv"

    def phase_pool(self, l):
        P, I = self.P, self.i
        wp = P.sb("wp", [128, 4, 128], BF16)
        psc = P.sb("psc", [128, 4], F32)
        P.dma("pool", wp.ap[:], I["w_pool"][l].rearrange("g c d -> c g d"), writes=[wp])
        P.dma("sp", psc.ap[:], I["psc"][l], writes=[psc])
        W = 512 + 16
        zs = [P.sb("z%d" % i, [128, W], BF16) for i in range(4)]
        sa = P.sb("sa", [128, W], F32); sbb = P.sb("sbb", [128, W], F32)
        ics = [P.sb("ic%d" % i, [128, 512], F32) for i in range(4)]
        dts_ = [P.sb("dd%d" % i, [128, 512], BF16) for i in range(2)]
        outs = [P.sb("po%d" % i, [128, 4, 512], BF16) for i in range(2)]
        nz = 0; nt = 0
        for (t0, TT, s) in tiles_for(l):
            s0, N = (0, S) if s == 0 else (S, L)
            lo = max(t0 - 8, s0); hi = min(t0 + TT + 8, s0 + N)
            j0 = lo - (t0 - 8); j1 = j0 + (hi - lo)
            WW = TT + 16
            rd = self.blk(self.rPT, lo, hi)
            po = outs[nt % 2]; nt += 1
            for gi in range(4):
                z = zs[gi]; ic = ics[gi]; dd = dts_[nz % 2]; nz += 1
                if j0 > 0 or j1 < WW:
                    P.op("pool", lambda e, z=z: e.memset(z.ap[:], 0.0), writes=[z])
                r0 = OFF_P + gi * 128
                P.dma("sp", z.ap[:, j0:j1], self.PT[r0:r0 + 128, lo:hi], reads=rd, writes=[z])
                P.dma("sp", ic.ap[:, 0:TT], I["invc"][:, gi, t0:t0 + TT], writes=[ic])
                P.op("dve", lambda e, z=z, WW=WW: e.tensor_tensor(out=sa.ap[:, 1:WW], in0=z.ap[:, 0:WW - 1], in1=z.ap[:, 1:WW], op=ALU.add),
                     reads=[z], writes=[sa])
                cur, nxt = sa, sbb
                lo_v, hi_v = 1, WW
                sh = 1
                for step in range(gi):
                    a, b_ = lo_v + sh, hi_v - sh
                    P.op("dve", lambda e, cur=cur, nxt=nxt, a=a, b_=b_, sh=sh: e.tensor_tensor(
                        out=nxt.ap[:, a:b_], in0=cur.ap[:, a - sh:b_ - sh], in1=cur.ap[:, a + sh:b_ + sh], op=ALU.add),
                        reads=[cur], writes=[nxt])
                    cur, nxt = nxt, cur
                    lo_v, hi_v = a, b_
                    sh *= 2
                assert lo_v <= 8 and hi_v >= 8 + TT
                P.op("dve", lambda e, cur=cur, TT=TT, ic=ic: e.tensor_tensor(out=cur.ap[:, 8:8 + TT], in0=cur.ap[:, 8:8 + TT], in1=ic.ap[:, 0:TT], op=ALU.mult),
                     reads=[cur, ic], writes=[cur])
                P.op("dve", lambda e, cur=cur, z=z, dd=dd, TT=TT: e.tensor_tensor(out=dd.ap[:, 0:TT], in0=cur.ap[:, 8:8 + TT], in1=z.ap[:, 8:8 + TT],
                                                                               op=ALU.subtract), reads=[cur, z], writes=[dd])
                ps = P.psum[6 + gi % 2]
                P.op("pe", lambda e, ps=ps, gi=gi, dd=dd, TT=TT: e.matmul(ps.ap[:, 0:TT], lhsT=wp.ap[:, gi, :], rhs=dd.ap[:, 0:TT], start=True, stop=True),
                     reads=[wp, dd], writes=[ps])
                P.op("act", lambda e, ps=ps, gi=gi, po=po, TT=TT: e.activation(out=po.ap[:, gi, 0:TT], in_=ps.ap[:, 0:TT], func=AF.Copy,
                                                                             scale=psc.ap[:, gi:gi + 1]), reads=[ps, psc], writes=[po])
            P.dma("act", self.DT[:, t0:t0 + TT].rearrange("(c p) t -> p c t", p=128), po.ap[:, :, 0:TT], reads=[po],
                  writes=self.blk(self.rDT, t0, t0 + TT))
            yield "pool"

    def phase_mix(self, l):
        self.P.phase_begin()
        ga, gc, gp = self.phase_attn(l), self.phase_conv(l), self.phase_pool(l)

        def step(g):
            try:
                return next(g)
            except StopIteration:
                return None

        while True:
            r = step(ga)
            ca = step(gc)
            pa = step(gp)
            cb_ = step(gc) if ca is not None else None
            if r != "kv":
                break
        while step(gc) is not None or step(gp) is not None:
            pass
        while step(ga) is not None:
            pass

    def outp_weights(self, l, load):
        P, I = self.P, self.i
        ws = [P.sb_top("wao", [128, 4, D], BF16), P.sb_top("wco", [128, 4, D], BF16), P.sb_top("wpo", [128, 4, D], BF16),
              P.sb_top("wout", [128, 8, D], BF16)]
        if load:
            for w_, nm, kc in zip(ws, ("w_attn_o", "w_conv_o", "w_pool_o", "w_out"), (4, 4, 4, 8)):
                self.load_cast_rows(w_, I[nm][l], D, None, kc)
        return ws

    def phase_outp(self, l):
        P, I = self.P, self.i
        P.phase_begin()
        wao, wco, wpo, wout = self._ow
        P.top_off = self._ow_top
        g1 = [self.load_mod("g1_%d" % s, s, M_G1) for s in range(2)]
        ats_ = [P.sb("at%d" % i, [128, 4, 512], BF16) for i in range(2)]
        sts_ = [P.sb("st%d" % i, [128, 4, 512], BF16) for i in range(2)]
        dts2 = [P.sb("dt%d" % i, [128, 4, 512], BF16) for i in range(2)]
        gts_ = [P.sb("gt%d" % i, [128, 24, 512], BF16) for i in range(2)]
        m1s = [P.sb("m1_%d" % i, [128, 512], F32) for i in range(2)]
        m2s = [P.sb("m2_%d" % i, [128, 512], F32) for i in range(2)]
        m3s = [P.sb("m3_%d" % i, [128, 512], F32) for i in range(2)]
        mgs = [P.sb("mg%d" % i, [128, 8, 512], BF16) for i in range(2)]
        xts = [P.sb("xo%d" % i, [128, D], F32) for i in range(2)]
        tmps = [P.sb("tmpo%d" % i, [128, 512], F32) for i in range(2)]
        nx = 0; npz = 0; ntile = 0; nfc = 0; ntmp = 0
        otiles = tiles_for(l)

        def load_tile(ti):
            (t0, TT, s) = otiles[ti]
            at = ats_[ti % 2]; st = sts_[ti % 2]; dt_ = dts2[ti % 2]; gt = gts_[ti % 2]
            P.dma("sp", at.ap[:, :, 0:TT], self.AT[:, t0:t0 + TT].rearrange("(c p) t -> p c t", p=128),
                  reads=self.blk(self.rAT, t0, t0 + TT), writes=[at])
            P.dma("sp", st.ap[:, :, 0:TT], self.ST[:, t0:t0 + TT].rearrange("(c p) t -> p c t", p=128),
                  reads=self.blk(self.rST, t0, t0 + TT), writes=[st])
            P.dma("sp", dt_.ap[:, :, 0:TT], self.DT[:, t0:t0 + TT].rearrange("(c p) t -> p c t", p=128),
                  reads=self.blk(self.rDT, t0, t0 + TT), writes=[dt_])
            P.dma("sp", gt.ap[:, :, 0:TT], self.PT[OFF_G:INW, t0:t0 + TT].rearrange("(c p) t -> p c t", p=128),
                  reads=self.blk(self.rPT, t0, t0 + TT), writes=[gt])

        load_tile(0)
        for (t0, TT, s) in otiles:
            at = ats_[ntile % 2]; st = sts_[ntile % 2]; dt_ = dts2[ntile % 2]; gt = gts_[ntile % 2]; mg = mgs[ntile % 2]
            ntile += 1
            if ntile < len(otiles):
                load_tile(ntile)
            for fc in range(8):
                pss = [P.psum[(npz * 3 + i) % 6] for i in range(3)]
                npz += 1
                for bi, (w_, src) in enumerate(((wao, at), (wco, st), (wpo, dt_))):
                    for k in range(4):
                        P.op("pe", lambda e, ps=pss[bi], w_=w_, src=src, k=k, fc=fc, TT=TT: e.matmul(
                            ps.ap[:, 0:TT], lhsT=w_.ap[:, k, fc * 128:(fc + 1) * 128], rhs=src.ap[:, k, 0:TT],
                            start=(k == 0), stop=(k == 3)), reads=[w_.part(k), src], writes=[pss[bi]])
                m1 = m1s[nfc % 2]; m2 = m2s[nfc % 2]; m3 = m3s[nfc % 2]; nfc += 1
                P.op("dve", lambda e, ps=pss[0], fc=fc, TT=TT, m1=m1, gt=gt: e.tensor_tensor(out=m1.ap[:, 0:TT], in0=ps.ap[:, 0:TT], in1=gt.ap[:, fc, 0:TT], op=ALU.mult),
                     reads=[pss[0], gt], writes=[m1])
                P.op("dve", lambda e, ps=pss[1], fc=fc, TT=TT, m2=m2, gt=gt: e.tensor_tensor(out=m2.ap[:, 0:TT], in0=ps.ap[:, 0:TT], in1=gt.ap[:, 8 + fc, 0:TT], op=ALU.mult),
                     reads=[pss[1], gt], writes=[m2])
                P.op("dve", lambda e, ps=pss[2], fc=fc, TT=TT, m3=m3, gt=gt: e.tensor_tensor(out=m3.ap[:, 0:TT], in0=ps.ap[:, 0:TT], in1=gt.ap[:, 16 + fc, 0:TT], op=ALU.mult),
                     reads=[pss[2], gt], writes=[m3])
                P.op("pool", lambda e, TT=TT, m1=m1, m2=m2: e.tensor_tensor(out=m1.ap[:, 0:TT], in0=m1.ap[:, 0:TT], in1=m2.ap[:, 0:TT], op=ALU.add),
                     reads=[m1, m2], writes=[m1])
                P.op("pool", lambda e, fc=fc, TT=TT, m1=m1, m3=m3, mg=mg: e.tensor_tensor(out=mg.ap[:, fc, 0:TT], in0=m1.ap[:, 0:TT], in1=m3.ap[:, 0:TT], op=ALU.add),
                     reads=[m1, m3], writes=[mg])
            for sub in range(TT // 128):
                xt = xts[nx % 2]; nx += 1
                r0 = t0 + sub * 128
                rx = self.blk(self.rXR, r0, r0 + 128)
                P.dma("sp", xt.ap[:], self.xrow_src(l, r0), reads=rx, writes=[xt])
                for half in range(2):
                    ps = P.psum[6 + half]
                    for fc in range(8):
                        P.op("pe", lambda e, ps=ps, fc=fc, sub=sub, half=half, mg=mg: e.matmul(
                            ps.ap[:, :], lhsT=mg.ap[:, fc, sub * 128:(sub + 1) * 128], rhs=wout.ap[:, fc, half * 512:(half + 1) * 512],
                            start=(fc == 0), stop=(fc == 7)), reads=[mg, wout.part(fc)], writes=[ps])
                    tmp = tmps[ntmp % 2]; ntmp += 1
                    P.op("dve", lambda e, ps=ps, half=half, s=s, tmp=tmp: e.tensor_tensor(out=tmp.ap[:], in0=ps.ap[:, :], in1=g1[s].ap[:, half * 512:(half + 1) * 512],
                                                                             op=ALU.mult), reads=[ps, g1[s]], writes=[tmp])
                    P.op("pool", lambda e, xt=xt, half=half, tmp=tmp: e.tensor_tensor(out=xt.ap[:, half * 512:(half + 1) * 512], in0=xt.ap[:, half * 512:(half + 1) * 512],
                                                                           in1=tmp.ap[:], op=ALU.add), reads=[xt, tmp], writes=[xt])
                P.dma("pool", self.XR[r0:r0 + 128, :], xt.ap[:], reads=[xt], writes=rx)

    def phase_route(self, l):
        P, I = self.P, self.i
        P.phase_begin()
        sets = [(0, 0, 32, 512)] + ([(1, S, 2, 32)] if l == 0 else [])
        pre = [P.sb_top("pw%s" % n, [128, 8, D], BF16) for n in ("g", "u", "d")]
        for n, nm in enumerate(("w_e_gate", "w_e_up", "w_e_down")):
            self.load_cast_rows(pre[n], I[nm][l, 0], D, None, 8)
        self._pre_wb0 = pre
        self._pre_top = P.top_off
        wr = P.sb("wr", [128, 8, NEXP], BF16)
        P.dma("pool", wr.ap[:], I["w_router"][l].rearrange("(k p) e -> p k e", p=128), writes=[wr])
        gs2 = {s: self.load_mod("gs2_%d" % s, s, M_GS2) for (s, _, _, _) in sets}
        sh2 = {s: self.load_mod("sh2_%d" % s, s, M_SH2) for (s, _, _, _) in sets}
        xts = [P.sb("xr%d" % i, [128, D], F32) for i in range(2)]
        hbs = [P.sb("hr%d" % i, [128, D], BF16) for i in range(2)]
        junk = P.sb("junkr", [128, D], BF16)
        sss = [P.sb("ssr%d" % i, [128, 2], F32) for i in range(2)]
        hTs = [P.sb("hTr%d" % i, [128, 8, 128], BF16) for i in range(2)]
        sms = [P.sb("sm%d" % i, [128, 4], F32) for i in range(2)]
        exs = [P.sb("ex%d" % i, [128, NEXP], F32) for i in range(2)]
        affs = [P.sb("aff%d" % i, [128, NEXP], F32) for i in range(2)]
        affT = P.sb("affT", [16, S], F32)
        junkb = P.sb("junkb", [16, S], BF16)
        bs = P.sb("bs", [16, 4], F32)
        G = P.sb("G", [128, 512], F32); mask = P.sb("mask", [128, 512], BF16)
        CS = P.sb("CS", [128, 512], F32); Pfx = P.sb("Pfx", [128, 512], F32)
        posm = P.sb("posm", [128, 512], F32)
        ghi = P.sb("ghi", [128, 512], BF16); gmid = P.sb("gmid", [128, 512], BF16); r1 = P.sb("r1", [128, 512], F32)
        vals = P.sb("vals", [128, 32, NEXP, 5], BF16)
        selB = [P.sb("selB%d" % i, [128, 32, 128], BF16) for i in range(2)]
        iv = P.sb("iv", [128, 20], F32)
        iv2 = P.sb("iv2", [128, 8], F32)
        sgf = P.sb("sgf", [128, 512], F32); oh = P.sb("oh", [128, 512], BF16)
        vals4 = P.sb("vals4", [128, 32, NEXP, 4, 5], BF16)
        nb_ = 0
        for (s, s0, nblk, cap) in sets:
            ntok = nblk * 128
            def stage_a(blk, n):
                xt = xts[n % 2]; hb = hbs[n % 2]; ss = sss[n % 2]; hT = hTs[n % 2]
                r0 = s0 + blk * 128
                P.dma("sp", xt.ap[:], self.XR[r0:r0 + 128, :], reads=self.blk(self.rXR, r0, r0 + 128), writes=[xt])
                self.norm_mod(xt, gs2[s], sh2[s], hb, ss, junk)
                P.dma("pool", self.H2[r0:r0 + 128, :], hb.ap[:], reads=[hb], writes=[self.rH2])
                self.transpose8(hb, P.psum[n % 2], hT.ap[:], hT)

            def stage_b(blk, n):
                hT = hTs[n % 2]; sm = sms[n % 2]; ex = exs[n % 2]; aff = affs[n % 2]
                psL = P.psum[2 + n % 2]
                for k in range(8):
                    P.op("pe", lambda e, k=k: e.matmul(psL.ap[:, 0:NEXP], lhsT=hT.ap[:, k, :], rhs=wr.ap[:, k, :],
                                                       start=(k == 0), stop=(k == 7)), reads=[hT, wr], writes=[psL])
                P.op("dve", lambda e: e.tensor_reduce(out=sm.ap[:, 0:1], in_=psL.ap[:, 0:NEXP], axis=AX.X, op=ALU.max),
                     reads=[psL], writes=[sm])
                P.op("dve", lambda e: e.tensor_scalar(out=sm.ap[:, 1:2], in0=sm.ap[:, 0:1], scalar1=-1.0, scalar2=None, op0=ALU.mult),
                     reads=[sm], writes=[sm])
                P.op("act", lambda e: e.activation(out=ex.ap[:], in_=psL.ap[:, 0:NEXP], func=AF.Exp, bias=sm.ap[:, 1:2],
                                                   accum_out=sm.ap[:, 2:3]), reads=[psL, sm], writes=[ex, sm])
                P.op("dve", lambda e: e.reciprocal(out=sm.ap[:, 3:4], in_=sm.ap[:, 2:3]), reads=[sm], writes=[sm])
                P.op("dve", lambda e: e.tensor_scalar(out=aff.ap[:], in0=ex.ap[:], scalar1=sm.ap[:, 3:4], scalar2=None, op0=ALU.mult),
                     reads=[ex, sm], writes=[aff])
                psX = P.psum[4 + n % 2]
                P.op("pe", lambda e: e.transpose(out=psX.ap[0:16, 0:128], in_=aff.ap[:, :], identity=self.identf.ap[:, :]),
                     reads=[aff, self.identf], writes=[psX])
                P.op("act", lambda e: e.copy(out=affT.ap[:, blk * 128:(blk + 1) * 128], in_=psX.ap[0:16, 0:128]),
                     reads=[psX], writes=[affT])

            stage_a(0, nb_)
            for blk in range(nblk):
                if blk + 1 < nblk:
                    stage_a(blk + 1, nb_ + 1)
                stage_b(blk, nb_)
                nb_ += 1
            P.op("dve", lambda e: e.memset(bs.ap[:], 0.0), writes=[bs])
            for it in range(24):
                cstep = 2.0 ** -(it + 1)
                P.op("dve", lambda e, cstep=cstep: e.tensor_scalar(out=bs.ap[:, 1:2], in0=bs.ap[:, 0:1], scalar1=cstep, scalar2=None, op0=ALU.add),
                     reads=[bs], writes=[bs])
                P.op("dve", lambda e, ntok=ntok: e.tensor_scalar(out=junkb.ap[:, 0:ntok], in0=affT.ap[:, 0:ntok], scalar1=bs.ap[:, 1:2], scalar2=None,
                                                             op0=ALU.is_ge, op1=ALU.add, accum_out=bs.ap[:, 2:3]),
                     reads=[affT, bs], writes=[junkb, bs])
                P.op("dve", lambda e, cstep=cstep, cap=cap: e.tensor_scalar(out=bs.ap[:, 3:4], in0=bs.ap[:, 2:3], scalar1=cap - 0.5, scalar2=cstep,
                                                                           op0=ALU.is_ge, op1=ALU.mult), reads=[bs], writes=[bs])
                P.op("dve", lambda e: e.tensor_tensor(out=bs.ap[:, 0:1], in0=bs.ap[:, 0:1], in1=bs.ap[:, 3:4], op=ALU.add),
                     reads=[bs], writes=[bs])
            P.op("dve", lambda e, ntok=ntok: e.scalar_tensor_tensor(out=affT.ap[:, 0:ntok], in0=affT.ap[:, 0:ntok], scalar=bs.ap[:, 0:1],
                                                                   in1=affT.ap[:, 0:ntok], op0=ALU.is_ge, op1=ALU.mult),
                 reads=[affT, bs], writes=[affT])
            NC_ = nblk * 16
            psG = P.psum[6]
            for blk in range(nblk):
                P.op("pe", lambda e, blk=blk: e.transpose(out=psG.ap[:, blk * 16:(blk + 1) * 16], in_=affT.ap[:, blk * 128:(blk + 1) * 128],
                                                          identity=self.identf.ap[0:16, 0:16]), reads=[affT, self.identf], writes=[psG])
            P.op("act", lambda e, NC_=NC_: e.copy(out=G.ap[:, 0:NC_], in_=psG.ap[:, 0:NC_]), reads=[psG], writes=[G])
            P.op("dve", lambda e, NC_=NC_: e.tensor_scalar(out=mask.ap[:, 0:NC_], in0=G.ap[:, 0:NC_], scalar1=0.0, scalar2=None, op0=ALU.is_gt),
                 reads=[G], writes=[mask])
            psPos = P.psum[7]; psCs = P.psum[5]
            P.op("pe", lambda e, NC_=NC_: e.matmul(psPos.ap[:, 0:NC_], lhsT=self.cb.ap[:, 2, :], rhs=mask.ap[:, 0:NC_], start=True, stop=True),
                 reads=[self.cb, mask], writes=[psPos])
            P.op("pe", lambda e, NC_=NC_: e.matmul(psCs.ap[:, 0:NC_], lhsT=self.cb.ap[:, 1, :], rhs=mask.ap[:, 0:NC_], start=True, stop=True),
                 reads=[self.cb, mask], writes=[psCs])
            P.op("act", lambda e, NC_=NC_: e.copy(out=CS.ap[:, 0:NC_], in_=psCs.ap[:, 0:NC_]), reads=[psCs], writes=[CS])
            P.op("dve", lambda e: e.memset(Pfx.ap[:, 0:16], 0.0), writes=[Pfx])
            for i in range(1, nblk):
                P.op("dve", lambda e, i=i: e.tensor_tensor(out=Pfx.ap[:, i * 16:(i + 1) * 16], in0=Pfx.ap[:, (i - 1) * 16:i * 16],
                                                           in1=CS.ap[:, (i - 1) * 16:i * 16], op=ALU.add), reads=[Pfx, CS], writes=[Pfx])
            P.op("dve", lambda e, NC_=NC_: e.tensor_tensor(out=posm.ap[:, 0:NC_], in0=psPos.ap[:, 0:NC_], in1=Pfx.ap[:, 0:NC_], op=ALU.add),
                 reads=[psPos, Pfx], writes=[posm])
            P.op("dve", lambda e, NC_=NC_: e.scalar_tensor_tensor(out=posm.ap[:, 0:NC_], in0=posm.ap[:, 0:NC_], scalar=1.0, in1=mask.ap[:, 0:NC_],
                                                                 op0=ALU.add, op1=ALU.mult), reads=[posm, mask], writes=[posm])
            P.op("dve", lambda e, NC_=NC_: e.tensor_scalar(out=posm.ap[:, 0:NC_], in0=posm.ap[:, 0:NC_], scalar1=-1.0, scalar2=None, op0=ALU.add),
                 reads=[posm], writes=[posm])
            P.op("dve", lambda e, NC_=NC_: e.tensor_copy(out=ghi.ap[:, 0:NC_], in_=G.ap[:, 0:NC_]), reads=[G], writes=[ghi])
            P.op("dve", lambda e, NC_=NC_: e.tensor_tensor(out=r1.ap[:, 0:NC_], in0=G.ap[:, 0:NC_], in1=ghi.ap[:, 0:NC_], op=ALU.subtract),
                 reads=[G, ghi], writes=[r1])
            P.op("dve", lambda e, NC_=NC_: e.tensor_copy(out=gmid.ap[:, 0:NC_], in_=r1.ap[:, 0:NC_]), reads=[r1], writes=[gmid])
            v4 = lambda t, NC_=NC_: t.ap[:, 0:NC_].rearrange("p (b e) -> p b e", e=16)
            P.op("dve", lambda e, o=vals.ap[:, 0:nblk, :, 2], i=v4(ghi): e.tensor_copy(out=o, in_=i), reads=[ghi], writes=[vals])
            P.op("dve", lambda e, o=vals.ap[:, 0:nblk, :, 3], i=v4(gmid): e.tensor_copy(out=o, in_=i), reads=[gmid], writes=[vals])
            P.op("dve", lambda e, NC_=NC_: e.tensor_tensor(out=r1.ap[:, 0:NC_], in0=r1.ap[:, 0:NC_], in1=gmid.ap[:, 0:NC_], op=ALU.subtract),
                 reads=[r1, gmid], writes=[r1])
            P.op("dve", lambda e, o=vals.ap[:, 0:nblk, :, 4], i=v4(r1): e.tensor_copy(out=o, in_=i), reads=[r1], writes=[vals])
            b0 = s0 // 128
            for ee in range(NEXP):
                P.op("dve", lambda e, o=vals.ap[:, 0:nblk, ee, 0:2], i=self.tokv.ap[:, b0:b0 + nblk, :]: e.tensor_copy(out=o, in_=i),
                     reads=[self.tokv], writes=[vals])
            npart = min(cap, 128)
            nsg = (cap + 127) // 128
            v3 = lambda t, NC_=NC_: t.ap[:, 0:NC_].rearrange("p (b e) -> p b e", e=16)
            if nsg > 1:
                P.op("dve", lambda e, NC_=NC_: e.tensor_scalar(out=sgf.ap[:, 0:NC_], in0=posm.ap[:, 0:NC_], scalar1=128.0, scalar2=None, op0=ALU.is_ge),
                     reads=[posm], writes=[sgf])
                for thr in (256.0, 384.0):
                    P.op("dve", lambda e, NC_=NC_, thr=thr: e.scalar_tensor_tensor(out=sgf.ap[:, 0:NC_], in0=posm.ap[:, 0:NC_], scalar=thr,
                                                                                  in1=sgf.ap[:, 0:NC_], op0=ALU.is_ge, op1=ALU.add),
                         reads=[posm, sgf], writes=[sgf])
                P.op("dve", lambda e, NC_=NC_: e.scalar_tensor_tensor(out=posm.ap[:, 0:NC_], in0=sgf.ap[:, 0:NC_], scalar=-128.0, in1=posm.ap[:, 0:NC_],
                                                                     op0=ALU.mult, op1=ALU.add), reads=[posm, sgf], writes=[posm])
                for sg in range(nsg):
                    P.op("dve", lambda e, NC_=NC_, sg=sg: e.tensor_scalar(out=oh.ap[:, 0:NC_], in0=sgf.ap[:, 0:NC_], scalar1=float(sg), scalar2=None,
                                                                         op0=ALU.is_equal), reads=[sgf], writes=[oh])
                    for c in range(5):
                        P.op("dve", lambda e, o=vals4.ap[:, 0:nblk, :, sg, c], i0_=vals.ap[:, 0:nblk, :, c], i1_=v3(oh): e.tensor_tensor(
                            out=o, in0=i0_, in1=i1_, op=ALU.mult), reads=[vals, oh], writes=[vals4])
            else:
                for c in range(5):
                    P.op("dve", lambda e, o=vals4.ap[:, 0:nblk, :, 0, c], i0_=vals.ap[:, 0:nblk, :, c]: e.tensor_copy(out=o, in_=i0_),
                         reads=[vals], writes=[vals4])
            iota_bc = self.iota.ap[:, 0:npart].unsqueeze(1).broadcast_to([128, nblk, npart])
            pos3 = posm.ap[:, 0:NC_].rearrange("p (b e) -> p b e", e=16)
            for ee in range(NEXP):
                ps = P.psum[ee % 4]
                sel = selB[ee % 2]
                en = "dve"
                P.op(en, lambda e, o=sel.ap[:, 0:nblk, 0:npart], i0_=iota_bc, i1_=pos3[:, :, ee].unsqueeze(2).broadcast_to([128, nblk, npart]):
                     e.tensor_tensor(out=o, in0=i0_, in1=i1_, op=ALU.is_equal), reads=[self.iota, posm], writes=[sel])
                for blk in range(nblk):
                    P.op("pe", lambda e, ps=ps, sel=sel, blk=blk, ee=ee, npart=npart, nsg=nsg, last=(blk == nblk - 1): e.matmul(
                        ps.ap[0:npart, 0:nsg * 5], lhsT=sel.ap[:, blk, 0:npart], rhs=vals4.ap[:, blk, ee, 0:nsg, :],
                        start=(blk == 0), stop=last), reads=[sel, vals4], writes=[ps])
                col0 = ee * 4 if s == 0 else 64 + ee
                P.op("act", lambda e, ps=ps, npart=npart, nsg=nsg: e.copy(out=iv.ap[0:npart, 0:nsg * 5], in_=ps.ap[0:npart, 0:nsg * 5]),
                     reads=[ps], writes=[iv])
                iv5 = iv.ap[0:npart, 0:nsg * 5].rearrange("p (g c) -> p g c", c=5)
                P.op("dve", lambda e, iv5=iv5, npart=npart, nsg=nsg: e.scalar_tensor_tensor(out=iv2.ap[0:npart, 0:nsg], in0=iv5[:, :, 0], scalar=64.0,
                                                                                       in1=iv5[:, :, 1], op0=ALU.mult, op1=ALU.add), reads=[iv], writes=[iv2])
                P.op("dve", lambda e, npart=npart, nsg=nsg, col0=col0: e.tensor_copy(out=self.IDX.ap[0:npart, col0:col0 + nsg], in_=iv2.ap[0:npart, 0:nsg]),
                     reads=[iv2], writes=[self.IDX])
                P.op("dve", lambda e, iv5=iv5, npart=npart, nsg=nsg: e.tensor_tensor(out=iv2.ap[0:npart, 4:4 + nsg], in0=iv5[:, :, 2], in1=iv5[:, :, 3],
                                                                                op=ALU.add), reads=[iv], writes=[iv2])
                P.op("dve", lambda e, iv5=iv5, npart=npart, nsg=nsg, col0=col0: e.tensor_tensor(out=self.GV.ap[0:npart, col0:col0 + nsg], in0=iv2.ap[0:npart, 4:4 + nsg],
                                                                                           in1=iv5[:, :, 4], op=ALU.add), reads=[iv, iv2], writes=[self.GV])
        if self.debug:
            idxd = self.nc.dram_tensor("IDXD", [128, 80], I32, kind="ExternalOutput").ap()
            gvd = self.nc.dram_tensor("GVD", [128, 80], F32, kind="ExternalOutput").ap()
            P.dma("sp", idxd[:, :], self.IDX.ap[:], reads=[self.IDX])
            P.dma("sp", gvd[:, :], self.GV.ap[:], reads=[self.GV])

    def phase_expert(self, l):
        P, I = self.P, self.i
        P.phase_begin()
        sets = [(0, 512, 128, 4)] + ([(1, 32, 32, 1)] if l == 0 else [])
        g2 = {s: self.load_mod("g2_%d" % s, s, M_G2) for (s, _, _, _) in sets}
        P.top_off = self._pre_top
        wb = [self._pre_wb0,
              [P.sb("w%s1" % n, [128, 8, D], BF16) for n in ("g", "u", "d")]]
        xes = [[P.sb("xe%d_%d" % (i, j), [128, D], BF16) for j in range(5)] for i in range(2)]
        xeT = P.sb("xeT", [128, 8, 512], BF16)
        sas = [P.sb("sae%d" % i, [128, 512], F32) for i in range(2)]
        hm = P.sb("hm", [128, 8, 512], BF16)
        yes = [P.sb("ye%d" % i, [128, D], F32) for i in range(2)]

        def loadw(e):
            for n, nm in enumerate(("w_e_gate", "w_e_up", "w_e_down")):
                self.load_cast_rows(wb[e % 2][n], I[nm][l, e], D, None, 8)

        def jobs(e):
            out = []
            for (s, nsel, npart, ngrp) in sets:
                for sg in range(ngrp):
                    col = (e * 4 + sg) if s == 0 else (64 + e)
                    out.append((s, sg, col, npart, xes[e % 2][sg if s == 0 else 4]))
            return out

        def gather(e):
            for (s, sg, col, npart, xe) in jobs(e):
                P._dma("pool", lambda q, xe=xe, npart=npart, col=col: q.indirect_dma_start(
                    out=xe.ap[0:npart, :], out_offset=None, in_=self.H2[:, :],
                    in_offset=bass.IndirectOffsetOnAxis(ap=self.IDX.ap[0:npart, col:col + 1], axis=0)),
                    [self.IDX, self.rH2], [xe])

        gather(0)
        ny = 0; nsa = 0; ntr = 0
        for e_ in range(NEXP):
            if e_ + 1 < NEXP:
                loadw(e_ + 1)
                gather(e_ + 1)
            wg, wu, wd = wb[e_ % 2]
            for (s, nsel, npart, ngrp) in sets:
                for sg in range(ngrp):
                    xe = xes[e_ % 2][sg if s == 0 else 4]
                    self.transpose8(xe, P.psum[ntr % 2], xeT.ap[:, :, sg * npart:(sg + 1) * npart], xeT, nrows=npart,
                                    eng=("act" if ntr % 2 else "dve"))
                    ntr += 1
                for fc in range(8):
                    psA = P.psum[2 + (fc % 2) * 2]; psU = P.psum[3 + (fc % 2) * 2]
                    for (ps, w_) in ((psA, wg), (psU, wu)):
                        for k in range(8):
                            P.op("pe", lambda e, ps=ps, w_=w_, k=k, fc=fc, nsel=nsel: e.matmul(
                                ps.ap[:, 0:nsel], lhsT=w_.ap[:, k, fc * 128:(fc + 1) * 128], rhs=xeT.ap[:, k, 0:nsel],
                                start=(k == 0), stop=(k == 7)), reads=[w_.part(k), xeT], writes=[ps])
                    sa = sas[nsa % 2]; nsa += 1
                    P.op("act", lambda e, psA=psA, nsel=nsel, sa=sa: e.activation(out=sa.ap[:, 0:nsel], in_=psA.ap[:, 0:nsel], func=AF.Silu),
                         reads=[psA], writes=[sa])
                    P.op("dve", lambda e, psU=psU, fc=fc, nsel=nsel, sa=sa: e.tensor_tensor(out=hm.ap[:, fc, 0:nsel], in0=psU.ap[:, 0:nsel], in1=sa.ap[:, 0:nsel],
                                                                                       op=ALU.mult), reads=[psU, sa], writes=[hm])
                for sg in range(ngrp):
                    col = (e_ * 4 + sg) if s == 0 else (64 + e_)
                    ye = yes[ny % 2]; ny += 1
                    for half in range(2):
                        ps = P.psum[6 + half]
                        for fc in range(8):
                            P.op("pe", lambda e, ps=ps, fc=fc, sg=sg, half=half, npart=npart, wd=wd: e.matmul(
                                ps.ap[0:npart, :], lhsT=hm.ap[:, fc, sg * npart:(sg + 1) * npart], rhs=wd.ap[:, fc, half * 512:(half + 1) * 512],
                                start=(fc == 0), stop=(fc == 7)), reads=[hm, wd.part(fc)], writes=[ps])
                        P.op("dve", lambda e, ps=ps, ye=ye, half=half, npart=npart, col=col, s=s: e.scalar_tensor_tensor(
                            out=ye.ap[0:npart, half * 512:(half + 1) * 512], in0=ps.ap[0:npart, :], scalar=self.GV.ap[0:npart, col:col + 1],
                            in1=g2[s].ap[0:npart, half * 512:(half + 1) * 512], op0=ALU.mult, op1=ALU.mult),
                            reads=[ps, self.GV, g2[s]], writes=[ye])
                    P._dma("pool", lambda q, ye=ye, npart=npart, col=col: q.indirect_dma_start(
                        out=self.XR[:, :], out_offset=bass.IndirectOffsetOnAxis(ap=self.IDX.ap[0:npart, col:col + 1], axis=0),
                        in_=ye.ap[0:npart, :], in_offset=None, compute_op=ALU.add, oob_is_err=True),
                        [ye, self.IDX], [self.rXRS])

    def phase_final(self):
        P, I = self.P, self.i
        P.phase_begin()
        fg = P.sb("fg", [128, D], F32)
        P.dma("sp", fg.ap[:], I["fng"][:, :], writes=[fg])
        xts = [P.sb("xf%d" % i, [128, D], F32) for i in range(2)]
        obs = [P.sb("of%d" % i, [128, D], F32) for i in range(2)]
        junk = P.sb("junkf", [128, D], BF16)
        ss = P.sb("ssf", [128, 2], F32)
        for i in range(32):
            xt = xts[i % 2]; ob = obs[i % 2]
            rx = list(self.blk(self.rXR, i * 128, (i + 1) * 128)) + [self.rXRS]
            P.dma("sp", xt.ap[:], self.XR[i * 128:(i + 1) * 128, :], reads=rx, writes=[xt])
            self.norm_mod(xt, fg, None, ob, ss, junk)
            P.dma("pool", self.out[i * 128:(i + 1) * 128, :], ob.ap[:], reads=[ob], writes=[])


def _bf(a):
    return np.asarray(a, np.float32).astype(ml_dtypes.bfloat16)


def host_consts():
    c = {}
    cb = np.zeros((128, 7, 128), np.float32)
    cb[:, 0, :] = np.eye(128)
    cb[:, 1, :] = 1.0
    tp = np.arange(128)
    cb[:, 2, :] = (tp[:, None] < tp[None, :]).astype(np.float32)
    cb[:, 3, :] = 1.0 / 512
    c["cb16"] = _bf(cb)
    mk = np.zeros((128, 2, 512), np.float32)
    j = np.arange(128)[:, None]; i = np.arange(128)[None, :]
    mk[:, 0, :] = np.tile((j >= i).astype(np.float32), (1, 4))
    mk[:, 1, :] = np.tile((j <= i).astype(np.float32), (1, 4))
    c["mk4"] = _bf(mk)
    c["identf"] = np.eye(128, dtype=np.float32)
    c["iota"] = np.tile(np.arange(512, dtype=np.float32)[None, :], (128, 1))
    t = (np.arange(34)[None, :] * 128 + np.arange(128)[:, None])
    c["tokv"] = _bf(np.stack([t // 64, t % 64], axis=-1))
    tt = np.arange(S)
    row = (tt // 64).astype(np.float64); col = (tt % 64).astype(np.float64)
    freqs = (10000.0 ** (-np.arange(16, dtype=np.float32) / np.float32(16))).astype(np.float32).astype(np.float64)
    ar = row[None, :] * freqs[:, None]; ac = col[None, :] * freqs[:, None]
    cos = np.concatenate([np.cos(ar), np.cos(ar), np.cos(ac), np.cos(ac)], 0)
    sin = np.concatenate([-np.sin(ar), np.sin(ar), -np.sin(ac), np.sin(ac)], 0)
    rope = np.zeros((4, 64, NT), np.float32)
    rope[0, :, :S] = cos; rope[0, :, S:] = 1.0
    rope[1, :, :S] = sin
    rope[2, :, :S] = cos / 8; rope[2, :, S:] = 1.0 / 8
    rope[3, :, :S] = sin / 8
    c["rope"] = rope
    invc = np.zeros((4, NT), np.float32)
    for gi, w in enumerate((2, 4, 8, 16)):
        for (s0, N) in ((0, S), (S, L)):
            tl = np.arange(N)
            lo = np.clip(tl - w // 2, 0, N); hi = np.clip(tl + w - w // 2, 0, N)
            invc[gi, s0:s0 + N] = (1.0 / (hi - lo).astype(np.float32))
    c["invc"] = np.ascontiguousarray(np.broadcast_to(invc[None], (128, 4, NT)))
    return c


def host_inputs(inp, b, consts):
    f = lambda a: np.ascontiguousarray(np.asarray(a, np.float32))
    rep = lambda v: np.ascontiguousarray(np.broadcast_to(np.asarray(v, np.float32)[None, :], (128, v.shape[-1])))
    pk = lambda v, n: np.ascontiguousarray(np.asarray(v, np.float32).reshape(n, 128).T)
    m = dict(consts)
    m["x"] = f(inp["x"][b]); m["ctx"] = f(inp["ctx"][b])
    m["cvec"] = np.ascontiguousarray(np.stack([pk(inp["c"][b], 8), pk(inp["c_ctx"], 8)], -1))
    m["ng"] = np.stack([np.stack([rep(inp["norm1_g"][l]), rep(inp["norm2_g"][l])]) for l in range(2)])
    m["fng"] = rep(inp["final_norm_g"])
    m["w_mod"] = f(inp["w_mod"]); m["bmod"] = np.stack([rep(inp["b_mod"][l]) for l in range(2)])
    m["w_in"] = f(inp["w_in"])
    m["sink"] = np.stack([rep(inp["attn_sink"][l]) for l in range(2)])
    m["w_attn_o"] = f(inp["w_attn_o"])
    m["dwt"] = np.stack([np.ascontiguousarray(np.asarray(inp["conv_dw"][l], np.float32).T.reshape(4, 128, 31).transpose(1, 0, 2))
                         for l in range(2)])
    m["cvp"] = np.stack([np.stack([pk(inp["conv_dw_b"][l], 4), pk(inp["conv_ln_g"][l], 4), pk(inp["conv_ln_b"][l], 4)], 1)
                         for l in range(2)])
    m["w_conv_o"] = f(inp["w_conv_o"]); m["w_pool"] = f(inp["w_pool"])
    m["psc"] = np.stack([pk(inp["pool_scale"][l], 4) for l in range(2)])
    m["w_pool_o"] = f(inp["w_pool_o"]); m["w_out"] = f(inp["w_out"]); m["w_router"] = f(inp["w_router"])
    m["w_e_gate"] = f(inp["w_e_gate"]); m["w_e_up"] = f(inp["w_e_up"]); m["w_e_down"] = f(inp["w_e_down"])
    return m


_CACHE = {}
NCORES = 4


def build(phases=None, debug=False):
    k = K(debug=debug)
    k.setup()
    for l in range(2):
        for name in ("mod", "inproj", "mix", "outp", "route", "expert"):
            if phases is not None and (l, name) not in phases:
                continue
            getattr(k, "phase_" + name)(l)
    if phases is None or "final" in phases:
        k.phase_final()
    k.P.finish()
    return k


def kernel(**inputs):
    inp = {n: np.asarray(v) for n, v in inputs.items()}
    if "k" not in _CACHE:
        _CACHE["k"] = build()
        _CACHE["c"] = host_consts()
    k = _CACHE["k"]
    maps = [host_inputs(inp, c % 4, _CACHE["c"]) for c in range(NCORES)]
    res = run_bass_kernel_spmd(k.nc, maps, core_ids=list(range(NCORES)))
    out = np.stack([np.asarray(res.results[b]["out"], np.float32) for b in range(4)])
    return out
```

```python
import contextlib
import numpy as np
import ml_dtypes
import concourse.bass as bass
import concourse.mybir as mybir
from concourse.bass_utils import run_bass_kernel_spmd

F32 = mybir.dt.float32
BF16 = mybir.dt.bfloat16
I32 = mybir.dt.int32
U32 = mybir.dt.uint32
AF = mybir.ActivationFunctionType
ALU = mybir.AluOpType
AX = mybir.AxisListType

_DTSIZE = {F32: 4, BF16: 2, I32: 4, U32: 4}


class Res:
    def __init__(self, name, ap=None):
        self.name = name
        self.ap = ap
        self.w = {}
        self.r = {}
        self.parts = {}

    def part(self, k, col=0):
        key = (k, col // 2048)
        if key not in self.parts:
            self.parts[key] = Res("%s.%d.%d" % (self.name, key[0], key[1]), self.ap)
        return self.parts[key]


class _Eng:
    def __init__(self, name, sem):
        self.name = name
        self.sem = sem
        self.count = 0
        self.seen = {}
        self.items = []
        self.slots = []
        self.slot_i = 0


class Prog:
    NSLOT = 8
    SB_LIMIT = 212992

    def __init__(self):
        self.nc = bass.Bass("TRN2", target_bir_lowering=False)
        nc = self.nc
        self.stack = contextlib.ExitStack()
        self.sems = {}
        self.eng = {}
        for n in ("pe", "act", "dve", "pool", "sp"):
            s = self.stack.enter_context(nc.semaphore("s_" + n))
            self.sems[id(s)] = s
            self.eng[n] = _Eng(n, s)
        for n in ("sp", "act", "pool"):
            for i in range(self.NSLOT * {"sp": 4, "pool": 3, "act": 2}[n]):
                s = self.stack.enter_context(nc.semaphore("d_%s%d" % (n, i)))
                self.sems[id(s)] = s
                self.eng[n].slots.append([s, 0])
        self.psum = []
        for i in range(8):
            t = self.stack.enter_context(nc.psum_tensor("ps%d" % i, [128, 512], F32))
            self.psum.append(Res("ps%d" % i, t))
        self.sb_base = 16640
        self.sb_off = 16640
        self.nalloc = 0

    def sb(self, name, shape, dtype, persist=False):
        n = 1
        for s in shape[1:]:
            n *= s
        nbytes = (n * _DTSIZE[dtype] + 63) // 64 * 64
        off = self.sb_off
        assert off + nbytes <= getattr(self, "top_off", self.SB_LIMIT), ("SBUF overflow", name, off, nbytes)
        self.nalloc += 1
        h = self.nc.alloc_sbuf_tensor_at("%s_%d" % (name, self.nalloc), list(shape), dtype, offset=off)
        self.sb_off = off + nbytes
        if persist:
            assert self.sb_base == off
            self.sb_base = self.sb_off
        return Res(name, h)

    def sb_top(self, name, shape, dtype):
        n = 1
        for s_ in shape[1:]:
            n *= s_
        nbytes = (n * _DTSIZE[dtype] + 63) // 64 * 64
        self.top_off = getattr(self, "top_off", self.SB_LIMIT) - nbytes
        assert self.top_off >= self.sb_off, ("SBUF overflow (top)", name)
        self.nalloc += 1
        h = self.nc.alloc_sbuf_tensor_at("%s_%d" % (name, self.nalloc), list(shape), dtype, offset=self.top_off)
        return Res(name, h)

    def _collect(self, e, reads, writes):
        need = {}

        def add(d):
            for k, v in d.items():
                if need.get(k, 0) < v:
                    need[k] = v

        for r in reads:
            add(r.w)
        for w in writes:
            add(w.w)
            add(w.r)
        waits = []
        for k, v in need.items():
            if e.name == "pe" and k == id(e.sem):
                continue
            if e.seen.get(k, 0) >= v:
                continue
            e.seen[k] = v
            waits.append((self.sems[k], v))
        return waits

    def _commit(self, key, val, reads, writes):
        for r in reads:
            if r.r.get(key, 0) < val:
                r.r[key] = val
        for w in writes:
            if w.w.get(key, 0) < val:
                w.w[key] = val
            w.r = {}

    def op(self, en, fn, reads=(), writes=()):
        e = self.eng[en]
        waits = self._collect(e, reads, writes)
        e.count += 1
        e.items.append((waits, fn, (e.sem, 1)))
        self._commit(id(e.sem), e.count, reads, writes)

    def dma(self, en, out, in_, reads=(), writes=(), **kw):
        self._dma(en, lambda q: q.dma_start(out=out, in_=in_, **kw), reads, writes)

    def _dma(self, en, fn, reads, writes):
        e = self.eng[en]
        slot = e.slots[e.slot_i]
        e.slot_i = (e.slot_i + 1) % len(e.slots)
        waits = self._collect(e, reads, writes)
        k = id(slot[0])
        if slot[1] > 0 and e.seen.get(k, 0) < 16 * slot[1]:
            e.seen[k] = 16 * slot[1]
            waits.append((slot[0], 16 * slot[1]))
        slot[1] += 1
        e.items.append((waits, fn, (slot[0], 16)))
        self._commit(k, 16 * slot[1], reads, writes)

    def barrier(self):
        allv = {}
        for e in self.eng.values():
            allv[id(e.sem)] = e.count
            for s, c in e.slots:
                allv[id(s)] = 16 * c
        for e in self.eng.values():
            waits = []
            for k, v in allv.items():
                if k == id(e.sem) and e.name == "pe":
                    continue
                if v > 0 and e.seen.get(k, 0) < v:
                    e.seen[k] = v
                    waits.append((self.sems[k], v))
            if waits:
                e.items.append((waits, None, None))

    def phase_begin(self):
        self.barrier()
        self.hw = max(getattr(self, "hw", 0), self.sb_off)
        self.sb_off = self.sb_base
        self.top_off = self.SB_LIMIT

    def finish(self):
        self.barrier()
        nc = self.nc
        with nc.Block() as block:
            def mk(en):
                def run(q):
                    for waits, fn, inc in self.eng[en].items:
                        for wi, (s, v) in enumerate(waits):
                            q.wait_ge(s, v)
                            if wi + 1 < len(waits) or fn is None:
                                q.nop()
                        if fn is not None:
                            ins = fn(q)
                            ins.then_inc(inc[0], inc[1])
                return run
            block.tensor(mk("pe"))
            block.scalar(mk("act"))
            block.vector(mk("dve"))
            block.gpsimd(mk("pool"))
            block.sync(mk("sp"))
        self.stack.close()


D = 1024
S = 4096
L = 256
NT = S + L
INW = 5376
NEXP = 16
EPS = 1e-6
OFF_Q, OFF_K, OFF_V, OFF_CA, OFF_CG, OFF_P, OFF_G = 0, 512, 640, 768, 1280, 1792, 2304
M_SH1, M_GS1, M_G1, M_SH2, M_GS2, M_G2 = range(6)


def tiles_for(layer, with_ctx_always=False):
    ts = [(i * 512, 512, 0) for i in range(8)]
    if layer == 0 or with_ctx_always:
        ts.append((S, L, 1))
    return ts


class K:
    def __init__(self, debug=False):
        self.P = Prog()
        self.debug = debug
        P = self.P
        nc = P.nc
        self.nc = nc

        def din(name, shape, dt=F32):
            return nc.dram_tensor(name, list(shape), dt, kind="ExternalInput").ap()

        def dscr(name, shape, dt):
            kind = "ExternalOutput"
            return nc.dram_tensor(name, list(shape), dt, kind=kind).ap()

        self.i = {}
        I = self.i
        I["x"] = din("x", [S, D]); I["ctx"] = din("ctx", [L, D])
        I["cvec"] = din("cvec", [128, 8, 2])
        I["ng"] = din("ng", [2, 2, 128, D]); I["fng"] = din("fng", [128, D])
        I["w_mod"] = din("w_mod", [2, D, 6 * D]); I["bmod"] = din("bmod", [2, 128, 6 * D])
        I["w_in"] = din("w_in", [2, D, INW])
        I["sink"] = din("sink", [2, 128, 8])
        I["w_attn_o"] = din("w_attn_o", [2, 512, D])
        I["dwt"] = din("dwt", [2, 128, 4, 31]); I["cvp"] = din("cvp", [2, 128, 3, 4])
        I["w_conv_o"] = din("w_conv_o", [2, 512, D])
        I["w_pool"] = din("w_pool", [2, 4, 128, 128]); I["psc"] = din("psc", [2, 128, 4])
        I["w_pool_o"] = din("w_pool_o", [2, 512, D]); I["w_out"] = din("w_out", [2, D, D])
        I["w_router"] = din("w_router", [2, D, NEXP])
        I["w_e_gate"] = din("w_e_gate", [2, NEXP, D, D]); I["w_e_up"] = din("w_e_up", [2, NEXP, D, D])
        I["w_e_down"] = din("w_e_down", [2, NEXP, D, D])
        I["cb16"] = din("cb16", [128, 7, 128], BF16)
        I["mk4"] = din("mk4", [128, 2, 512], BF16)
        I["identf"] = din("identf", [128, 128]); I["iota"] = din("iota", [128, 512])
        I["tokv"] = din("tokv", [128, 34, 2], BF16)
        I["rope"] = din("rope", [4, 64, NT])
        I["invc"] = din("invc", [128, 4, NT])
        self.out = nc.dram_tensor("out", [S, D], F32, kind="ExternalOutput").ap()
        self.XR = dscr("XR", [NT, D], F32)
        self.PT = dscr("PT", [INW, NT], BF16)
        self.AT = dscr("AT", [512, NT], BF16)
        self.ST = dscr("ST", [512, NT], BF16)
        self.DT = dscr("DT", [512, NT], BF16)
        self.H2 = dscr("H2", [NT, D], BF16)
        self.MODD = dscr("MODD", [2, 6, 128, D], F32)
        nb = NT // 128
        self.rXR = [Res("XR%d" % i) for i in range(nb)]
        self.rPT = [Res("PT%d" % i) for i in range(nb)]
        self.rAT = [Res("AT%d" % i) for i in range(nb)]
        self.rST = [Res("ST%d" % i) for i in range(nb)]
        self.rDT = [Res("DT%d" % i) for i in range(nb)]
        self.rH2 = Res("H2")
        self.rMODD = Res("MODD")
        self.rXRS = Res("XRS")

    def xrow_src(self, l, r0):
        if l == 0:
            if r0 < S:
                return self.i["x"][r0:r0 + 128, :]
            return self.i["ctx"][r0 - S:r0 - S + 128, :]
        return self.XR[r0:r0 + 128, :]

    def blk(self, rl, t0, t1):
        t0 = max(t0, 0); t1 = min(t1, NT)
        return rl[t0 // 128:(t1 + 127) // 128]

    def setup(self):
        P, I = self.P, self.i
        self.cb = P.sb("cb16", [128, 7, 128], BF16, persist=True)
        self.mk4 = P.sb("mk4", [128, 2, 512], BF16, persist=True)
        self.identf = P.sb("identf", [128, 128], F32, persist=True)
        self.iota = P.sb("iota", [128, 512], F32, persist=True)
        self.tokv = P.sb("tokv", [128, 34, 2], BF16, persist=True)
        self.IDX = P.sb("IDX", [128, 80], I32, persist=True)
        self.GV = P.sb("GV", [128, 80], F32, persist=True)
        self.onesf = P.sb("onesf", [128, 128], F32, persist=True)
        P.dma("sp", self.cb.ap[:], I["cb16"][:, :, :], writes=[self.cb])
        P.dma("sp", self.mk4.ap[:], I["mk4"][:, :, :], writes=[self.mk4])
        P.dma("sp", self.identf.ap[:], I["identf"][:, :], writes=[self.identf])
        P.dma("sp", self.iota.ap[:], I["iota"][:, :], writes=[self.iota])
        P.dma("sp", self.tokv.ap[:], I["tokv"][:, :, :], writes=[self.tokv])
        P.op("dve", lambda e: e.memset(self.onesf.ap[:], 1.0), writes=[self.onesf])
        P.op("dve", lambda e: e.memset(self.IDX.ap[:], 0), writes=[self.IDX])
        P.op("dve", lambda e: e.memset(self.GV.ap[:], 0.0), writes=[self.GV])

    def ident(self):
        return self.cb.ap[:, 0, :]

    def ones(self):
        return self.cb.ap[:, 1, :]

    def load_cast_rows(self, dst, src_rows_ap, ncols, writes, kchunks):
        P = self.P
        for c0 in range(0, ncols, 2048):
            c1 = min(c0 + 2048, ncols)
            for k in range(kchunks):
                P.dma("pool", dst.ap[:, k, c0:c1], src_rows_ap[k * 128:(k + 1) * 128, c0:c1], writes=[dst.part(k, c0)])

    def phase_mod(self, l):
        P, I = self.P, self.i
        P.phase_begin()
        cv = P.sb("cv", [128, 8, 2], F32)
        sc = P.sb("sc", [128, 8, 2], F32)
        screp = P.sb("screp", [128, 2, 8, 128], BF16)
        wms = [P.sb("wm%d" % i, [128, 8, 3072], BF16) for i in range(2)]
        bms = [P.sb("bm%d" % i, [128, 3072], F32) for i in range(2)]
        ngt = P.sb("ngt", [128, 2, D], F32)
        mos = [P.sb("mo%d" % i, [128, 2, 3072], F32) for i in range(2)]
        P.dma("sp", cv.ap[:], I["cvec"][:, :, :], writes=[cv])
        P.dma("sp", ngt.ap[:], I["ng"][l].rearrange("n p d -> p n d"), writes=[ngt])
        for half in range(2):
            c0 = half * 3072
            self.load_cast_rows(wms[half], I["w_mod"][l][:, c0:c0 + 3072], 3072, None, 8)
            P.dma("sp", bms[half].ap[:], I["bmod"][l][:, c0:c0 + 3072], writes=[bms[half]])
        P.op("act", lambda e: e.activation(out=sc.ap[:], in_=cv.ap[:], func=AF.Silu), reads=[cv], writes=[sc])
        for s in range(2):
            for k in range(8):
                P.op("dve", lambda e, s=s, k=k: e.tensor_scalar(
                    out=screp.ap[:, s, k, :], in0=self.onesf.ap[:], scalar1=sc.ap[:, k, s:s + 1], scalar2=None,
                    op0=ALU.mult), reads=[sc, self.onesf], writes=[screp])
        npz = 0
        for half in range(2):
            wm = wms[half]; bm = bms[half]; mo = mos[half]
            for s in range(2):
                for j in range(6):
                    ps = P.psum[npz % 8]; npz += 1
                    for k in range(8):
                        P.op("pe", lambda e, ps=ps, s=s, k=k, j=j, wm=wm: e.matmul(
                            ps.ap[:, :], lhsT=screp.ap[:, s, k, :], rhs=wm.ap[:, k, j * 512:(j + 1) * 512],
                            start=(k == 0), stop=(k == 7)), reads=[screp, wm.part(k, j * 512)], writes=[ps])
                    P.op("dve", lambda e, ps=ps, s=s, j=j, bm=bm, mo=mo: e.tensor_tensor(
                        out=mo.ap[:, s, j * 512:(j + 1) * 512], in0=ps.ap[:, :], in1=bm.ap[:, j * 512:(j + 1) * 512],
                        op=ALU.add), reads=[ps, bm], writes=[mo])
            for s in range(2):
                P.op("dve", lambda e, s=s, half=half, mo=mo: e.scalar_tensor_tensor(
                    out=mo.ap[:, s, 1024:2048], in0=mo.ap[:, s, 1024:2048], scalar=1.0, in1=ngt.ap[:, half, :],
                    op0=ALU.add, op1=ALU.mult), reads=[mo, ngt], writes=[mo])
                P.dma("sp", self.MODD[s, half * 3:(half + 1) * 3].rearrange("c p d -> p c d"),
                      mo.ap[:, s, :].rearrange("p (c d) -> p c d", c=3), reads=[mo], writes=[self.rMODD])

    def load_mod(self, name, s, chunk):
        P = self.P
        t = P.sb(name, [128, D], F32)
        P.dma("sp", t.ap[:], self.MODD[s, chunk], reads=[self.rMODD], writes=[t])
        return t

    def norm_mod(self, xt, gs, sh, hb, ss, tmp):
        P = self.P
        P.op("act", lambda e: e.activation(out=tmp.ap[:], in_=xt.ap[:], func=AF.Square, accum_out=ss.ap[:, 0:1]),
             reads=[xt], writes=[tmp, ss])
        P.op("dve", lambda e: e.tensor_scalar(out=ss.ap[:, 1:2], in0=ss.ap[:, 0:1], scalar1=1.0 / D, scalar2=EPS,
                                              op0=ALU.mult, op1=ALU.add), reads=[ss], writes=[ss])
        P.op("act", lambda e: e.activation(out=ss.ap[:, 1:2], in_=ss.ap[:, 1:2], func=AF.Ln), reads=[ss], writes=[ss])
        P.op("act", lambda e: e.activation(out=ss.ap[:, 1:2], in_=ss.ap[:, 1:2], func=AF.Exp, scale=-0.5), reads=[ss], writes=[ss])
        if sh is None:
            P.op("dve", lambda e: e.scalar_tensor_tensor(out=hb.ap[:], in0=xt.ap[:], scalar=ss.ap[:, 1:2], in1=gs.ap[:],
                                                         op0=ALU.mult, op1=ALU.mult), reads=[xt, ss, gs], writes=[hb])
            return
        P.op("dve", lambda e: e.scalar_tensor_tensor(out=xt.ap[:], in0=xt.ap[:], scalar=ss.ap[:, 1:2], in1=gs.ap[:],
                                                     op0=ALU.mult, op1=ALU.mult), reads=[xt, ss, gs], writes=[xt])
        P.op("dve", lambda e: e.tensor_tensor(out=hb.ap[:], in0=xt.ap[:], in1=sh.ap[:], op=ALU.add),
             reads=[xt, sh], writes=[hb])

    def transpose8(self, hb, ps, dst_ap, dst_res, nrows=128, eng="act"):
        P = self.P
        pv = ps.ap[:, :].bitcast(BF16)
        for k in range(8):
            P.op("pe", lambda e, k=k: e.transpose(out=pv[:, k * 128:k * 128 + nrows], in_=hb.ap[0:nrows, k * 128:(k + 1) * 128],
                                                  identity=self.cb.ap[0:nrows, 0, 0:nrows]),
                 reads=[hb, self.cb], writes=[ps])
        src = pv.rearrange("p (k t) -> p k t", k=8)[:, :, 0:nrows]
        if eng == "act":
            P.op("act", lambda e: e.copy(out=dst_ap, in_=src), reads=[ps], writes=[dst_res])
        else:
            P.op("dve", lambda e: e.tensor_copy(out=dst_ap, in_=src), reads=[ps], writes=[dst_res])

    def phase_inproj(self, l):
        P, I = self.P, self.i
        P.phase_begin()
        win = P.sb("win", [128, 8, INW], BF16)
        gsx = [self.load_mod("gs%d" % s, s, M_GS1) for s in range(2)]
        shx = [self.load_mod("sh%d" % s, s, M_SH1) for s in range(2)]
        self.load_cast_rows(win, I["w_in"][l], INW, [win], 8)
        xts = [P.sb("xt%d" % i, [128, D], F32) for i in range(2)]
        hbs = [P.sb("hb%d" % i, [128, D], BF16) for i in range(2)]
        junk = P.sb("junk", [128, D], BF16)
        sss = [P.sb("ss%d" % i, [128, 2], F32) for i in range(2)]
        hTs = [P.sb("hT%d" % i, [128, 8, 512], BF16) for i in range(2)]
        obs = [P.sb("ob%d" % i, [128, 7, 512], BF16) for i in range(2)]
        st = {"nsub": 0, "ngrp": 0, "npj": 0}
        tiles = tiles_for(l, True)

        def prep(ti):
            (t0, TT, s) = tiles[ti]
            hT = hTs[ti % 2]
            for sub in range(TT // 128):
                n = st["nsub"]; st["nsub"] += 1
                xt = xts[n % 2]; hb = hbs[n % 2]; ss = sss[n % 2]
                r0 = t0 + sub * 128
                P.dma("sp", xt.ap[:], self.xrow_src(l, r0), reads=self.blk(self.rXR, r0, r0 + 128), writes=[xt])
                self.norm_mod(xt, gsx[s], shx[s], hb, ss, junk)
                self.transpose8(hb, P.psum[n % 2], hT.ap[:, :, sub * 128:(sub + 1) * 128], hT)

        def proj(ti, groups):
            (t0, TT, s) = tiles[ti]
            hT = hTs[ti % 2]
            for grp in groups:
                ob = obs[st["ngrp"] % 2]
                st["ngrp"] += 1
                for ci, c in enumerate(grp):
                    ps = P.psum[2 + st["npj"] % 6]
                    st["npj"] += 1
                    for k in range(8):
                        P.op("pe", lambda e, ps=ps, k=k, c=c, TT=TT, hT=hT: e.matmul(
                            ps.ap[:, 0:TT], lhsT=win.ap[:, k, c * 128:(c + 1) * 128], rhs=hT.ap[:, k, 0:TT],
                            start=(k == 0), stop=(k == 7)), reads=[win.part(k, c * 128), hT], writes=[ps])
                    sig = (c >= 18) or (10 <= c < 14)
                    if sig:
                        P.op("act", lambda e, ps=ps, ci=ci, TT=TT, ob=ob: e.activation(
                            out=ob.ap[:, ci, 0:TT], in_=ps.ap[:, 0:TT], func=AF.Sigmoid), reads=[ps], writes=[ob])
                    else:
                        P.op("dve", lambda e, ps=ps, ci=ci, TT=TT, ob=ob: e.tensor_copy(
                            out=ob.ap[:, ci, 0:TT], in_=ps.ap[:, 0:TT]), reads=[ps], writes=[ob])
                c0, n = grp[0], len(grp)
                P.dma("pool", self.PT[c0 * 128:(c0 + n) * 128, t0:t0 + TT].rearrange("(c p) t -> p c t", p=128),
                      ob.ap[:, 0:n, 0:TT], reads=[ob], writes=self.blk(self.rPT, t0, t0 + TT))

        prep(0)
        for ti, (t0, TT, s) in enumerate(tiles):
            if s == 1 and l == 1:
                groups = [[4, 5]]
            else:
                groups = [list(range(g * 7, g * 7 + 7)) for g in range(6)]
            h = len(groups) // 2
            proj(ti, groups[:h])
            if ti + 1 < len(tiles):
                prep(ti + 1)
            proj(ti, groups[h:])

    def rope(self, dst_ap, dst_res, raw_ap, swp_ap, cos_ap, sin_ap, t1, t2, reads, npart=64):
        P = self.P
        P.op("dve", lambda e: e.tensor_tensor(out=t1.ap[0:npart, 0:cos_ap.shape[-1]], in0=raw_ap, in1=cos_ap, op=ALU.mult),
             reads=reads, writes=[t1])
        P.op("dve", lambda e: e.tensor_tensor(out=t2.ap[0:npart, 0:cos_ap.shape[-1]], in0=swp_ap, in1=sin_ap, op=ALU.mult),
             reads=reads, writes=[t2])
        P.op("dve", lambda e: e.tensor_tensor(out=dst_ap, in0=t1.ap[0:npart, 0:cos_ap.shape[-1]],
                                              in1=t2.ap[0:npart, 0:cos_ap.shape[-1]], op=ALU.add),
             reads=[t1, t2], writes=[dst_res])

    def phase_attn(self, l):
        P, I = self.P, self.i
        PERM = [1, 0, 3, 2]
        kT = P.sb("kT", [64, 2, NT], BF16)
        vtok = P.sb("vtok", [128, 34, 128], BF16)
        snk = P.sb("snk", [128, 8], F32)
        esk = P.sb("esk", [64, 2, 512], F32)
        t1 = P.sb("t1", [64, 512], F32)
        t2 = P.sb("t2", [64, 512], F32)
        cosk = P.sb("cosk", [64, 512], F32); sink_ = P.sb("sink_", [64, 512], F32)
        kraws = [P.sb("kraw%d" % i, [64, 512], BF16) for i in range(2)]
        kswps = [P.sb("kswp%d" % i, [64, 512], BF16) for i in range(2)]
        vT = P.sb("vT", [128, 512], BF16)
        P.dma("sp", snk.ap[:], I["sink"][l], writes=[snk])
        P.op("act", lambda e: e.activation(out=snk.ap[:], in_=snk.ap[:], func=AF.Exp), reads=[snk], writes=[snk])
        for g in range(2):
            for h in range(4):
                P.op("dve", lambda e, g=g, h=h: e.tensor_scalar(
                    out=esk.ap[:, g, h * 128:(h + 1) * 128], in0=self.onesf.ap[0:64, :], scalar1=snk.ap[0:64, g * 4 + h:g * 4 + h + 1],
                    scalar2=None, op0=ALU.mult), reads=[snk, self.onesf], writes=[esk])
        nv = 0
        attn_mark = P.sb_off
        for (t0, TT, s) in tiles_for(l, True):
            rd = self.blk(self.rPT, t0, t0 + TT)
            P.dma("sp", cosk.ap[:, 0:TT], I["rope"][0, :, t0:t0 + TT], writes=[cosk])
            P.dma("sp", sink_.ap[:, 0:TT], I["rope"][1, :, t0:t0 + TT], writes=[sink_])
            for g in range(2):
                kraw = kraws[g]; kswp = kswps[g]
                base = OFF_K + g * 64
                P.dma("sp", kraw.ap[:, 0:TT], self.PT[base:base + 64, t0:t0 + TT], reads=rd, writes=[kraw])
                for j in range(4):
                    P.dma("sp", kswp.ap[j * 16:(j + 1) * 16, 0:TT],
                          self.PT[base + PERM[j] * 16:base + PERM[j] * 16 + 16, t0:t0 + TT], reads=rd, writes=[kswp])
                self.rope(kT.ap[:, g, t0:t0 + TT], kT, kraw.ap[:, 0:TT], kswp.ap[:, 0:TT], cosk.ap[:, 0:TT], sink_.ap[:, 0:TT],
                          t1, t2, [kraw, kswp, cosk, sink_])
            P.dma("sp", vT.ap[:, 0:TT], self.PT[OFF_V:OFF_V + 128, t0:t0 + TT], reads=rd, writes=[vT])
            for sub in range(TT // 128):
                ps = P.psum[nv % 2]
                nv += 1
                pv = ps.ap[:, :].bitcast(BF16)
                P.op("pe", lambda e, pv=pv, sub=sub: e.transpose(out=pv[:, 0:128], in_=vT.ap[:, sub * 128:(sub + 1) * 128],
                                                                 identity=self.ident()), reads=[vT, self.cb], writes=[ps])
                P.op("act", lambda e, pv=pv, b=(t0 // 128 + sub): e.copy(out=vtok.ap[:, b, :], in_=pv[:, 0:128]),
                     reads=[ps], writes=[vtok])
            yield "kv"
        yield "kvdone"
        P.barrier()
        P.hw = max(getattr(P, "hw", 0), P.sb_off)
        P.sb_off = attn_mark
        self._ow = self.outp_weights(l, load=True)
        self._ow_top = P.top_off
        qraws = [P.sb("qraw%d" % i, [64, 4, 512], BF16) for i in range(2)]
        qswps = [P.sb("qswp%d" % i, [64, 4, 512], BF16) for i in range(2)]
        qrs = [P.sb("qr%d" % i, [64, 4, 512], BF16) for i in range(2)]
        cosqs = [P.sb("cosq%d" % i, [64, 512], F32) for i in range(2)]
        sinqs = [P.sb("sinq%d" % i, [64, 512], F32) for i in range(2)]
        t1b = P.sb("t1b", [64, 512], F32); t2b = P.sb("t2b", [64, 512], F32)
        ets = [P.sb("et%d" % i, [128, 512], BF16) for i in range(6)]
        dens = [P.sb("den%d" % i, [64, 512], F32) for i in range(2)]
        ats = [P.sb("at%d" % i, [64, 512], BF16) for i in range(2)]
        LA = 4
        groups = [(ti, t0, TT, s_, g) for ti, (t0, TT, s_) in enumerate(tiles_for(l)) for g in range(2)]

        def prologue(gi):
            (ti, t0, TT, s_, g) = groups[gi]
            rd = self.blk(self.rPT, t0, t0 + TT)
            cosq = cosqs[ti % 2]; sinq = sinqs[ti % 2]
            qraw = qraws[gi % 2]; qswp = qswps[gi % 2]; qr = qrs[gi % 2]
            if g == 0:
                P.dma("sp", cosq.ap[:, 0:TT], I["rope"][2, :, t0:t0 + TT], writes=[cosq])
                P.dma("sp", sinq.ap[:, 0:TT], I["rope"][3, :, t0:t0 + TT], writes=[sinq])
            qrows = self.PT[g * 256:(g + 1) * 256, t0:t0 + TT]
            P.dma("sp", qraw.ap[:, :, 0:TT], qrows.rearrange("(h d) t -> d h t", d=64), reads=rd, writes=[qraw])
            q4 = qrows.rearrange("(h j d) t -> j d h t", j=4, d=16)
            for j in range(4):
                P.dma("sp", qswp.ap[j * 16:(j + 1) * 16, :, 0:TT], q4[PERM[j]], reads=rd, writes=[qswp])
            for h in range(4):
                self.rope(qr.ap[:, h, 0:TT], qr, qraw.ap[:, h, 0:TT], qswp.ap[:, h, 0:TT], cosq.ap[:, 0:TT], sinq.ap[:, 0:TT],
                          t1b, t2b, [qraw, qswp, cosq, sinq])

        cnt = {"S": 0, "Q": 0}

        def emit_S(step):
            (qr, g, qb, kb, mk, psN, psD, first, last, unit) = step["a"]
            psS = P.psum[cnt["S"] % 4]; et = ets[cnt["S"] % len(ets)]; cnt["S"] += 1
            step["et"] = et
            P.op("pe", lambda e: e.matmul(psS.ap[:, :], lhsT=kT.ap[:, g, kb * 128:(kb + 1) * 128], rhs=qr.ap[:, :, qb * 128:(qb + 1) * 128],
                                          start=True, stop=True), reads=[kT, qr], writes=[psS])
            P.op("act", lambda e: e.activation(out=et.ap[:], in_=psS.ap[:, :], func=AF.Exp), reads=[psS], writes=[et])
            if mk is not None:
                P.op("dve", lambda e: e.tensor_tensor(out=et.ap[:], in0=et.ap[:], in1=self.mk4.ap[:, mk, :], op=ALU.mult),
                     reads=[et, self.mk4], writes=[et])

        def emit_PV(step):
            (qr, g, qb, kb, mk, psN, psD, first, last, unit) = step["a"]
            et = step["et"]
            P.op("pe", lambda e: e.matmul(psN.ap[0:64, :], lhsT=vtok.ap[:, kb, g * 64:(g + 1) * 64], rhs=et.ap[:], start=first, stop=last),
                 reads=[vtok, et], writes=[psN])
            P.op("pe", lambda e: e.matmul(psD.ap[0:64, :], lhsT=self.cb.ap[:, 1, 0:64], rhs=et.ap[:], start=first, stop=last),
                 reads=[self.cb, et], writes=[psD])
            if last:
                (nq, c0) = unit
                at = ats[nq % 2]; den = dens[nq % 2]
                P.op("dve", lambda e: e.tensor_tensor(out=den.ap[:], in0=psD.ap[0:64, :], in1=esk.ap[:, g, :], op=ALU.add),
                     reads=[psD, esk], writes=[den])
                P.op("act", lambda e: e.activation(out=den.ap[:], in_=den.ap[:], func=AF.Ln), reads=[den], writes=[den])
                P.op("act", lambda e: e.activation(out=den.ap[:], in_=den.ap[:], func=AF.Exp, scale=-1.0), reads=[den], writes=[den])
                P.op("dve", lambda e: e.tensor_tensor(out=at.ap[:], in0=psN.ap[0:64, :], in1=den.ap[:], op=ALU.mult),
                     reads=[psN, den], writes=[at])
                P.dma("pool", self.AT[g * 256:(g + 1) * 256, c0:c0 + 128].rearrange("(h d) t -> d h t", d=64),
                      at.ap[:].rearrange("d (h t) -> d h t", h=4), reads=[at], writes=self.blk(self.rAT, c0, c0 + 128))

        prologue(0)
        for gi, (ti, t0, TT, s_, g) in enumerate(groups):
            qr = qrs[gi % 2]
            steps = []
            for qb in range(TT // 128):
                n = t0 // 128 + qb
                if s_ == 0:
                    kbs = []
                    if n > 0:
                        kbs.append((n - 1, 0))
                    kbs.append((n, None))
                    if n < 31:
                        kbs.append((n + 1, 1))
                    kbs += [(32, None), (33, None)]
                else:
                    kbs = [(32, None), (33, None)]
                nq = cnt["Q"]; cnt["Q"] += 1
                psN = P.psum[4 + nq % 2]; psD = P.psum[6 + nq % 2]
                for i, (kb, mk) in enumerate(kbs):
                    steps.append({"a": (qr, g, qb, kb, mk, psN, psD, i == 0, i == len(kbs) - 1, (nq, t0 + qb * 128))})
            mid = len(steps) // 2
            for j in range(len(steps) + LA):
                if j < len(steps):
                    emit_S(steps[j])
                if j == mid and gi + 1 < len(groups):
                    prologue(gi + 1)
                if j >= LA:
                    emit_PV(steps[j - LA])
            if g == 1:
                yield "attn"

    def phase_conv(self, l):
        P, I = self.P, self.i
        dwt = P.sb("dwt", [128, 4, 31], F32)
        cvp = P.sb("cvp", [128, 3, 4], F32)
        diag = P.sb("diag", [128, 4, 31, 128], BF16)
        P.dma("sp", dwt.ap[:], I["dwt"][l], writes=[dwt])
        P.dma("sp", cvp.ap[:], I["cvp"][l], writes=[cvp])
        identf32 = self.identf
        for c in range(4):
            P.op("dve", lambda e, o=diag.ap[:, c, :, :], i0_=identf32.ap[:, :].unsqueeze(1).broadcast_to([128, 31, 128]),
                 i1_=dwt.ap[:, c, :].unsqueeze(2).broadcast_to([128, 31, 128]): e.tensor_tensor(out=o, in0=i0_, in1=i1_, op=ALU.mult),
                 reads=[dwt, identf32], writes=[diag])
        W = 512 + 30
        uas = [P.sb("ua%d" % i, [128, W], BF16) for i in range(4)]
        ugs = [P.sb("ug%d" % i, [128, W], BF16) for i in range(4)]
        us = [P.sb("u%d" % i, [128, W], BF16) for i in range(4)]
        cvs = [P.sb("cvv%d" % i, [128, 4, 512], F32) for i in range(2)]
        vbs = [P.sb("vb%d" % i, [128, 4, 512], BF16) for i in range(1)] * 2
        vsqs = [P.sb("vsq%d" % i, [128, 4, 512], BF16) for i in range(1)] * 2
        mean = P.sb("mean", [128, 512], F32)
        rstd = P.sb("rstd", [128, 512], F32)
        yts = [P.sb("yt%d" % i, [128, 512], F32) for i in range(4)]
        sts = [P.sb("st%d" % i, [128, 4, 512], BF16) for i in range(2)]
        nu = 0; nt = 0
        for (t0, TT, s) in tiles_for(l):
            cv = cvs[nt % 2]; vb = vbs[nt % 2]; vsq = vsqs[nt % 2]
            s0, N = (0, S) if s == 0 else (S, L)
            lo = max(t0 - 15, s0); hi = min(t0 + TT + 15, s0 + N)
            j0 = lo - (t0 - 15); j1 = j0 + (hi - lo)
            WW = TT + 30
            rd = self.blk(self.rPT, lo, hi)
            for c in range(4):
                ua, ug, u = uas[nu % 4], ugs[nu % 4], us[nu % 4]
                nu += 1
                if j0 > 0 or j1 < WW:
                    P.op("pool", lambda e, cv=cv, vb=vb, vsq=vsq, ua=ua: e.memset(ua.ap[:], 0.0), writes=[ua])
                    P.op("pool", lambda e, cv=cv, vb=vb, vsq=vsq, ug=ug: e.memset(ug.ap[:], 0.0), writes=[ug])
                ra = OFF_CA + c * 128; rg = OFF_CG + c * 128
                P.dma("sp", ua.ap[:, j0:j1], self.PT[ra:ra + 128, lo:hi], reads=rd, writes=[ua])
                P.dma("sp", ug.ap[:, j0:j1], self.PT[rg:rg + 128, lo:hi], reads=rd, writes=[ug])
                P.op("pool", lambda e, cv=cv, vb=vb, vsq=vsq, ua=ua, ug=ug, u=u, WW=WW: e.tensor_tensor(out=u.ap[:, 0:WW], in0=ua.ap[:, 0:WW], in1=ug.ap[:, 0:WW],
                                                                         op=ALU.mult), reads=[ua, ug], writes=[u])
                ps = P.psum[2 + c % 2]
                for k in range(31):
                    P.op("pe", lambda e, cv=cv, vb=vb, vsq=vsq, ps=ps, c=c, k=k, u=u, TT=TT: e.matmul(
                        ps.ap[:, 0:TT], lhsT=diag.ap[:, c, k, :], rhs=u.ap[:, k:k + TT], start=(k == 0), stop=(k == 30)),
                        reads=[diag, u], writes=[ps])
                P.op("act", lambda e, cv=cv, vb=vb, vsq=vsq, ps=ps, c=c, TT=TT: e.activation(out=cv.ap[:, c, 0:TT], in_=ps.ap[:, 0:TT], func=AF.Identity,
                                                                     bias=cvp.ap[:, 0, c:c + 1]), reads=[ps, cvp], writes=[cv])
                P.op("act", lambda e, cv=cv, vb=vb, vsq=vsq, ps=ps, c=c, TT=TT: e.activation(out=vsq.ap[:, c, 0:TT], in_=ps.ap[:, 0:TT], func=AF.Square,
                                                                     bias=cvp.ap[:, 0, c:c + 1]), reads=[ps, cvp], writes=[vsq])
                P.op("act", lambda e, cv=cv, vb=vb, vsq=vsq, ps=ps, c=c, TT=TT: e.activation(out=vb.ap[:, c, 0:TT], in_=ps.ap[:, 0:TT], func=AF.Identity,
                                                                     bias=cvp.ap[:, 0, c:c + 1]), reads=[ps, cvp], writes=[vb])
            psM = P.psum[4]; psQ = P.psum[5]
            for c in range(4):
                P.op("pe", lambda e, cv=cv, vb=vb, vsq=vsq, c=c, TT=TT: e.matmul(psM.ap[:, 0:TT], lhsT=self.cb.ap[:, 3, :], rhs=vb.ap[:, c, 0:TT],
                                                          start=(c == 0), stop=(c == 3)), reads=[self.cb, vb], writes=[psM])
            for c in range(4):
                P.op("pe", lambda e, cv=cv, vb=vb, vsq=vsq, c=c, TT=TT: e.matmul(psQ.ap[:, 0:TT], lhsT=self.cb.ap[:, 3, :], rhs=vsq.ap[:, c, 0:TT],
                                                          start=(c == 0), stop=(c == 3)), reads=[self.cb, vsq], writes=[psQ])
            yield "convA"
            P.op("act", lambda e, cv=cv, vb=vb, vsq=vsq, TT=TT: e.copy(out=mean.ap[:, 0:TT], in_=psM.ap[:, 0:TT]), reads=[psM], writes=[mean])
            P.op("dve", lambda e, cv=cv, vb=vb, vsq=vsq, TT=TT: e.tensor_tensor(out=rstd.ap[:, 0:TT], in0=mean.ap[:, 0:TT], in1=mean.ap[:, 0:TT], op=ALU.mult),
                 reads=[mean], writes=[rstd])
            P.op("dve", lambda e, cv=cv, vb=vb, vsq=vsq, TT=TT: e.tensor_tensor(out=rstd.ap[:, 0:TT], in0=psQ.ap[:, 0:TT], in1=rstd.ap[:, 0:TT], op=ALU.subtract),
                 reads=[psQ, rstd], writes=[rstd])
            P.op("dve", lambda e, cv=cv, vb=vb, vsq=vsq, TT=TT: e.tensor_scalar(out=rstd.ap[:, 0:TT], in0=rstd.ap[:, 0:TT], scalar1=0.0, scalar2=EPS,
                                                         op0=ALU.max, op1=ALU.add), reads=[rstd], writes=[rstd])
            P.op("act", lambda e, cv=cv, vb=vb, vsq=vsq, TT=TT: e.activation(out=rstd.ap[:, 0:TT], in_=rstd.ap[:, 0:TT], func=AF.Sqrt), reads=[rstd], writes=[rstd])
            P.op("dve", lambda e, cv=cv, vb=vb, vsq=vsq, TT=TT: e.reciprocal(out=rstd.ap[:, 0:TT], in_=rstd.ap[:, 0:TT]), reads=[rstd], writes=[rstd])
            st = sts[nt % 2]; nt += 1
            for c in range(4):
                yt = yts[c]
                P.op("dve", lambda e, cv=cv, vb=vb, vsq=vsq, c=c, TT=TT, yt=yt: e.tensor_tensor(out=yt.ap[:, 0:TT], in0=cv.ap[:, c, 0:TT], in1=mean.ap[:, 0:TT], op=ALU.subtract),
                     reads=[cv, mean], writes=[yt])
                P.op("dve", lambda e, cv=cv, vb=vb, vsq=vsq, TT=TT, yt=yt: e.tensor_tensor(out=yt.ap[:, 0:TT], in0=yt.ap[:, 0:TT], in1=rstd.ap[:, 0:TT], op=ALU.mult),
                     reads=[yt, rstd], writes=[yt])
                P.op("act", lambda e, cv=cv, vb=vb, vsq=vsq, c=c, TT=TT, st=st, yt=yt: e.activation(out=st.ap[:, c, 0:TT], in_=yt.ap[:, 0:TT], func=AF.Silu,
                                                                     scale=cvp.ap[:, 1, c:c + 1], bias=cvp.ap[:, 2, c:c + 1]),
                     reads=[yt, cvp], writes=[st])
            P.dma("act", self.ST[:, t0:t0 + TT].rearrange("(c p) t -> p c t", p=128), st.ap[:, :, 0:TT], reads=[st],
                  writes=self.blk(self.rST, t0, t0 + TT))
            yield "conv"

    def phase_pool(self, l):
        P, I = self.P, self.i
        wp = P.sb("wp", [128, 4, 128], BF16)
        psc = P.sb("psc", [128, 4], F32)
        P.dma("pool", wp.ap[:], I["w_pool"][l].rearrange("g c d -> c g d"), writes=[wp])
        P.dma("sp", psc.ap[:], I["psc"][l], writes=[psc])
        W = 512 + 16
        zs = [P.sb("z%d" % i, [128, W], BF16) for i in range(4)]
        sa = P.sb("sa", [128, W], F32); sbb = P.sb("sbb", [128, W], F32)
        ics = [P.sb("ic%d" % i, [128, 512], F32) for i in range(4)]
        dts_ = [P.sb("dd%d" % i, [128, 512], BF16) for i in range(2)]
        outs = [P.sb("po%d" % i, [128, 4, 512], BF16) for i in range(2)]
        nz = 0; nt = 0
        for (t0, TT, s) in tiles_for(l):
            s0, N = (0, S) if s == 0 else (S, L)
            lo = max(t0 - 8, s0); hi = min(t0 + TT + 8, s0 + N)
            j0 = lo - (t0 - 8); j1 = j0 + (hi - lo)
            WW = TT + 16
            rd = self.blk(self.rPT, lo, hi)
            po = outs[nt % 2]; nt += 1
            for gi in range(4):
                z = zs[gi]; ic = ics[gi]; dd = dts_[nz % 2]; nz += 1
                if j0 > 0 or j1 < WW:
                    P.op("pool", lambda e, z=z: e.memset(z.ap[:], 0.0), writes=[z])
                r0 = OFF_P + gi * 128
                P.dma("sp", z.ap[:, j0:j1], self.PT[r0:r0 + 128, lo:hi], reads=rd, writes=[z])
                P.dma("sp", ic.ap[:, 0:TT], I["invc"][:, gi, t0:t0 + TT], writes=[ic])
                P.op("dve", lambda e, z=z, WW=WW: e.tensor_tensor(out=sa.ap[:, 1:WW], in0=z.ap[:, 0:WW - 1], in1=z.ap[:, 1:WW], op=ALU.add),
                     reads=[z], writes=[sa])
                cur, nxt = sa, sbb
                lo_v, hi_v = 1, WW
                sh = 1
                for step in range(gi):
                    a, b_ = lo_v + sh, hi_v - sh
                    P.op("dve", lambda e, cur=cur, nxt=nxt, a=a, b_=b_, sh=sh: e.tensor_tensor(
                        out=nxt.ap[:, a:b_], in0=cur.ap[:, a - sh:b_ - sh], in1=cur.ap[:, a + sh:b_ + sh], op=ALU.add),
                        reads=[cur], writes=[nxt])
                    cur, nxt = nxt, cur
                    lo_v, hi_v = a, b_
                    sh *= 2
                assert lo_v <= 8 and hi_v >= 8 + TT
                P.op("dve", lambda e, cur=cur, TT=TT, ic=ic: e.tensor_tensor(out=cur.ap[:, 8:8 + TT], in0=cur.ap[:, 8:8 + TT], in1=ic.ap[:, 0:TT], op=ALU.mult),
                     reads=[cur, ic], writes=[cur])
                P.op("dve", lambda e, cur=cur, z=z, dd=dd, TT=TT: e.tensor_tensor(out=dd.ap[:, 0:TT], in0=cur.ap[:, 8:8 + TT], in1=z.ap[:, 8:8 + TT],
                                                                               op=ALU.subtract), reads=[cur, z], writes=[dd])
                ps = P.psum[6 + gi % 2]
                P.op("pe", lambda e, ps=ps, gi=gi, dd=dd, TT=TT: e.matmul(ps.ap[:, 0:TT], lhsT=wp.ap[:, gi, :], rhs=dd.ap[:, 0:TT], start=True, stop=True),
                     reads=[wp, dd], writes=[ps])
                P.op("act", lambda e, ps=ps, gi=gi, po=po, TT=TT: e.activation(out=po.ap[:, gi, 0:TT], in_=ps.ap[:, 0:TT], func=AF.Copy,
                                                                             scale=psc.ap[:, gi:gi + 1]), reads=[ps, psc], writes=[po])
            P.dma("act", self.DT[:, t0:t0 + TT].rearrange("(c p) t -> p c t", p=128), po.ap[:, :, 0:TT], reads=[po],
                  writes=self.blk(self.rDT, t0, t0 + TT))
            yield "pool"

    def phase_mix(self, l):
        self.P.phase_begin()
        ga, gc, gp = self.phase_attn(l), self.phase_conv(l), self.phase_pool(l)

        def step(g):
            try:
                return next(g)
            except StopIteration:
                return None

        while True:
            r = step(ga)
            ca = step(gc)
            pa = step(gp)
            cb_ = step(gc) if ca is not None else None
            if r != "kv":
                break
        while step(gc) is not None or step(gp) is not None:
            pass
        while step(ga) is not None:
            pass

    def outp_weights(self, l, load):
        P, I = self.P, self.i
        ws = [P.sb_top("wao", [128, 4, D], BF16), P.sb_top("wco", [128, 4, D], BF16), P.sb_top("wpo", [128, 4, D], BF16),
              P.sb_top("wout", [128, 8, D], BF16)]
        if load:
            for w_, nm, kc in zip(ws, ("w_attn_o", "w_conv_o", "w_pool_o", "w_out"), (4, 4, 4, 8)):
                self.load_cast_rows(w_, I[nm][l], D, None, kc)
        return ws

    def phase_outp(self, l):
        P, I = self.P, self.i
        P.phase_begin()
        wao, wco, wpo, wout = self._ow
        P.top_off = self._ow_top
        g1 = [self.load_mod("g1_%d" % s, s, M_G1) for s in range(2)]
        ats_ = [P.sb("at%d" % i, [128, 4, 512], BF16) for i in range(2)]
        sts_ = [P.sb("st%d" % i, [128, 4, 512], BF16) for i in range(2)]
        dts2 = [P.sb("dt%d" % i, [128, 4, 512], BF16) for i in range(2)]
        gts_ = [P.sb("gt%d" % i, [128, 24, 512], BF16) for i in range(2)]
        m1s = [P.sb("m1_%d" % i, [128, 512], F32) for i in range(2)]
        m2s = [P.sb("m2_%d" % i, [128, 512], F32) for i in range(2)]
        m3s = [P.sb("m3_%d" % i, [128, 512], F32) for i in range(2)]
        mgs = [P.sb("mg%d" % i, [128, 8, 512], BF16) for i in range(2)]
        xts = [P.sb("xo%d" % i, [128, D], F32) for i in range(2)]
        tmps = [P.sb("tmpo%d" % i, [128, 512], F32) for i in range(2)]
        nx = 0; npz = 0; ntile = 0; nfc = 0; ntmp = 0
        otiles = tiles_for(l)

        def load_tile(ti):
            (t0, TT, s) = otiles[ti]
            at = ats_[ti % 2]; st = sts_[ti % 2]; dt_ = dts2[ti % 2]; gt = gts_[ti % 2]
            P.dma("sp", at.ap[:, :, 0:TT], self.AT[:, t0:t0 + TT].rearrange("(c p) t -> p c t", p=128),
                  reads=self.blk(self.rAT, t0, t0 + TT), writes=[at])
            P.dma("sp", st.ap[:, :, 0:TT], self.ST[:, t0:t0 + TT].rearrange("(c p) t -> p c t", p=128),
                  reads=self.blk(self.rST, t0, t0 + TT), writes=[st])
            P.dma("sp", dt_.ap[:, :, 0:TT], self.DT[:, t0:t0 + TT].rearrange("(c p) t -> p c t", p=128),
                  reads=self.blk(self.rDT, t0, t0 + TT), writes=[dt_])
            P.dma("sp", gt.ap[:, :, 0:TT], self.PT[OFF_G:INW, t0:t0 + TT].rearrange("(c p) t -> p c t", p=128),
                  reads=self.blk(self.rPT, t0, t0 + TT), writes=[gt])

        load_tile(0)
        for (t0, TT, s) in otiles:
            at = ats_[ntile % 2]; st = sts_[ntile % 2]; dt_ = dts2[ntile % 2]; gt = gts_[ntile % 2]; mg = mgs[ntile % 2]
            ntile += 1
            if ntile < len(otiles):
                load_tile(ntile)
            for fc in range(8):
                pss = [P.psum[(npz * 3 + i) % 6] for i in range(3)]
                npz += 1
                for bi, (w_, src) in enumerate(((wao, at), (wco, st), (wpo, dt_))):
                    for k in range(4):
                        P.op("pe", lambda e, ps=pss[bi], w_=w_, src=src, k=k, fc=fc, TT=TT: e.matmul(
                            ps.ap[:, 0:TT], lhsT=w_.ap[:, k, fc * 128:(fc + 1) * 128], rhs=src.ap[:, k, 0:TT],
                            start=(k == 0), stop=(k == 3)), reads=[w_.part(k), src], writes=[pss[bi]])
                m1 = m1s[nfc % 2]; m2 = m2s[nfc % 2]; m3 = m3s[nfc % 2]; nfc += 1
                P.op("dve", lambda e, ps=pss[0], fc=fc, TT=TT, m1=m1, gt=gt: e.tensor_tensor(out=m1.ap[:, 0:TT], in0=ps.ap[:, 0:TT], in1=gt.ap[:, fc, 0:TT], op=ALU.mult),
                     reads=[pss[0], gt], writes=[m1])
                P.op("dve", lambda e, ps=pss[1], fc=fc, TT=TT, m2=m2, gt=gt: e.tensor_tensor(out=m2.ap[:, 0:TT], in0=ps.ap[:, 0:TT], in1=gt.ap[:, 8 + fc, 0:TT], op=ALU.mult),
                     reads=[pss[1], gt], writes=[m2])
                P.op("dve", lambda e, ps=pss[2], fc=fc, TT=TT, m3=m3, gt=gt: e.tensor_tensor(out=m3.ap[:, 0:TT], in0=ps.ap[:, 0:TT], in1=gt.ap[:, 16 + fc, 0:TT], op=ALU.mult),
                     reads=[pss[2], gt], writes=[m3])
                P.op("pool", lambda e, TT=TT, m1=m1, m2=m2: e.tensor_tensor(out=m1.ap[:, 0:TT], in0=m1.ap[:, 0:TT], in1=m2.ap[:, 0:TT], op=ALU.add),
                     reads=[m1, m2], writes=[m1])
                P.op("pool", lambda e, fc=fc, TT=TT, m1=m1, m3=m3, mg=mg: e.tensor_tensor(out=mg.ap[:, fc, 0:TT], in0=m1.ap[:, 0:TT], in1=m3.ap[:, 0:TT], op=ALU.add),
                     reads=[m1, m3], writes=[mg])
            for sub in range(TT // 128):
                xt = xts[nx % 2]; nx += 1
                r0 = t0 + sub * 128
                rx = self.blk(self.rXR, r0, r0 + 128)
                P.dma("sp", xt.ap[:], self.xrow_src(l, r0), reads=rx, writes=[xt])
                for half in range(2):
                    ps = P.psum[6 + half]
                    for fc in range(8):
                        P.op("pe", lambda e, ps=ps, fc=fc, sub=sub, half=half, mg=mg: e.matmul(
                            ps.ap[:, :], lhsT=mg.ap[:, fc, sub * 128:(sub + 1) * 128], rhs=wout.ap[:, fc, half * 512:(half + 1) * 512],
                            start=(fc == 0), stop=(fc == 7)), reads=[mg, wout.part(fc)], writes=[ps])
                    tmp = tmps[ntmp % 2]; ntmp += 1
                    P.op("dve", lambda e, ps=ps, half=half, s=s, tmp=tmp: e.tensor_tensor(out=tmp.ap[:], in0=ps.ap[:, :], in1=g1[s].ap[:, half * 512:(half + 1) * 512],
                                                                             op=ALU.mult), reads=[ps, g1[s]], writes=[tmp])
                    P.op("pool", lambda e, xt=xt, half=half, tmp=tmp: e.tensor_tensor(out=xt.ap[:, half * 512:(half + 1) * 512], in0=xt.ap[:, half * 512:(half + 1) * 512],
                                                                           in1=tmp.ap[:], op=ALU.add), reads=[xt, tmp], writes=[xt])
                P.dma("pool", self.XR[r0:r0 + 128, :], xt.ap[:], reads=[xt], writes=rx)

    def phase_route(self, l):
        P, I = self.P, self.i
        P.phase_begin()
        sets = [(0, 0, 32, 512)] + ([(1, S, 2, 32)] if l == 0 else [])
        pre = [P.sb_top("pw%s" % n, [128, 8, D], BF16) for n in ("g", "u", "d")]
        for n, nm in enumerate(("w_e_gate", "w_e_up", "w_e_down")):
            self.load_cast_rows(pre[n], I[nm][l, 0], D, None, 8)
        self._pre_wb0 = pre
        self._pre_top = P.top_off
        wr = P.sb("wr", [128, 8, NEXP], BF16)
        P.dma("pool", wr.ap[:], I["w_router"][l].rearrange("(k p) e -> p k e", p=128), writes=[wr])
        gs2 = {s: self.load_mod("gs2_%d" % s, s, M_GS2) for (s, _, _, _) in sets}
        sh2 = {s: self.load_mod("sh2_%d" % s, s, M_SH2) for (s, _, _, _) in sets}
        xts = [P.sb("xr%d" % i, [128, D], F32) for i in range(2)]
        hbs = [P.sb("hr%d" % i, [128, D], BF16) for i in range(2)]
        junk = P.sb("junkr", [128, D], BF16)
        sss = [P.sb("ssr%d" % i, [128, 2], F32) for i in range(2)]
        hTs = [P.sb("hTr%d" % i, [128, 8, 128], BF16) for i in range(2)]
        sms = [P.sb("sm%d" % i, [128, 4], F32) for i in range(2)]
        exs = [P.sb("ex%d" % i, [128, NEXP], F32) for i in range(2)]
        affs = [P.sb("aff%d" % i, [128, NEXP], F32) for i in range(2)]
        affT = P.sb("affT", [16, S], F32)
        junkb = P.sb("junkb", [16, S], BF16)
        bs = P.sb("bs", [16, 4], F32)
        G = P.sb("G", [128, 512], F32); mask = P.sb("mask", [128, 512], BF16)
        CS = P.sb("CS", [128, 512], F32); Pfx = P.sb("Pfx", [128, 512], F32)
        posm = P.sb("posm", [128, 512], F32)
        ghi = P.sb("ghi", [128, 512], BF16); gmid = P.sb("gmid", [128, 512], BF16); r1 = P.sb("r1", [128, 512], F32)
        vals = P.sb("vals", [128, 32, NEXP, 5], BF16)
        selB = [P.sb("selB%d" % i, [128, 32, 128], BF16) for i in range(2)]
        iv = P.sb("iv", [128, 20], F32)
        iv2 = P.sb("iv2", [128, 8], F32)
        sgf = P.sb("sgf", [128, 512], F32); oh = P.sb("oh", [128, 512], BF16)
        vals4 = P.sb("vals4", [128, 32, NEXP, 4, 5], BF16)
        nb_ = 0
        for (s, s0, nblk, cap) in sets:
            ntok = nblk * 128
            def stage_a(blk, n):
                xt = xts[n % 2]; hb = hbs[n % 2]; ss = sss[n % 2]; hT = hTs[n % 2]
                r0 = s0 + blk * 128
                P.dma("sp", xt.ap[:], self.XR[r0:r0 + 128, :], reads=self.blk(self.rXR, r0, r0 + 128), writes=[xt])
                self.norm_mod(xt, gs2[s], sh2[s], hb, ss, junk)
                P.dma("pool", self.H2[r0:r0 + 128, :], hb.ap[:], reads=[hb], writes=[self.rH2])
                self.transpose8(hb, P.psum[n % 2], hT.ap[:], hT)

            def stage_b(blk, n):
                hT = hTs[n % 2]; sm = sms[n % 2]; ex = exs[n % 2]; aff = affs[n % 2]
                psL = P.psum[2 + n % 2]
                for k in range(8):
                    P.op("pe", lambda e, k=k: e.matmul(psL.ap[:, 0:NEXP], lhsT=hT.ap[:, k, :], rhs=wr.ap[:, k, :],
                                                       start=(k == 0), stop=(k == 7)), reads=[hT, wr], writes=[psL])
                P.op("dve", lambda e: e.tensor_reduce(out=sm.ap[:, 0:1], in_=psL.ap[:, 0:NEXP], axis=AX.X, op=ALU.max),
                     reads=[psL], writes=[sm])
                P.op("dve", lambda e: e.tensor_scalar(out=sm.ap[:, 1:2], in0=sm.ap[:, 0:1], scalar1=-1.0, scalar2=None, op0=ALU.mult),
                     reads=[sm], writes=[sm])
                P.op("act", lambda e: e.activation(out=ex.ap[:], in_=psL.ap[:, 0:NEXP], func=AF.Exp, bias=sm.ap[:, 1:2],
                                                   accum_out=sm.ap[:, 2:3]), reads=[psL, sm], writes=[ex, sm])
                P.op("dve", lambda e: e.reciprocal(out=sm.ap[:, 3:4], in_=sm.ap[:, 2:3]), reads=[sm], writes=[sm])
                P.op("dve", lambda e: e.tensor_scalar(out=aff.ap[:], in0=ex.ap[:], scalar1=sm.ap[:, 3:4], scalar2=None, op0=ALU.mult),
                     reads=[ex, sm], writes=[aff])
                psX = P.psum[4 + n % 2]
                P.op("pe", lambda e: e.transpose(out=psX.ap[0:16, 0:128], in_=aff.ap[:, :], identity=self.identf.ap[:, :]),
                     reads=[aff, self.identf], writes=[psX])
                P.op("act", lambda e: e.copy(out=affT.ap[:, blk * 128:(blk + 1) * 128], in_=psX.ap[0:16, 0:128]),
                     reads=[psX], writes=[affT])

            stage_a(0, nb_)
            for blk in range(nblk):
                if blk + 1 < nblk:
                    stage_a(blk + 1, nb_ + 1)
                stage_b(blk, nb_)
                nb_ += 1
            P.op("dve", lambda e: e.memset(bs.ap[:], 0.0), writes=[bs])
            for it in range(24):
                cstep = 2.0 ** -(it + 1)
                P.op("dve", lambda e, cstep=cstep: e.tensor_scalar(out=bs.ap[:, 1:2], in0=bs.ap[:, 0:1], scalar1=cstep, scalar2=None, op0=ALU.add),
                     reads=[bs], writes=[bs])
                P.op("dve", lambda e, ntok=ntok: e.tensor_scalar(out=junkb.ap[:, 0:ntok], in0=affT.ap[:, 0:ntok], scalar1=bs.ap[:, 1:2], scalar2=None,
                                                             op0=ALU.is_ge, op1=ALU.add, accum_out=bs.ap[:, 2:3]),
                     reads=[affT, bs], writes=[junkb, bs])
                P.op("dve", lambda e, cstep=cstep, cap=cap: e.tensor_scalar(out=bs.ap[:, 3:4], in0=bs.ap[:, 2:3], scalar1=cap - 0.5, scalar2=cstep,
                                                                           op0=ALU.is_ge, op1=ALU.mult), reads=[bs], writes=[bs])
                P.op("dve", lambda e: e.tensor_tensor(out=bs.ap[:, 0:1], in0=bs.ap[:, 0:1], in1=bs.ap[:, 3:4], op=ALU.add),
                     reads=[bs], writes=[bs])
            P.op("dve", lambda e, ntok=ntok: e.scalar_tensor_tensor(out=affT.ap[:, 0:ntok], in0=affT.ap[:, 0:ntok], scalar=bs.ap[:, 0:1],
                                                                   in1=affT.ap[:, 0:ntok], op0=ALU.is_ge, op1=ALU.mult),
                 reads=[affT, bs], writes=[affT])
            NC_ = nblk * 16
            psG = P.psum[6]
            for blk in range(nblk):
                P.op("pe", lambda e, blk=blk: e.transpose(out=psG.ap[:, blk * 16:(blk + 1) * 16], in_=affT.ap[:, blk * 128:(blk + 1) * 128],
                                                          identity=self.identf.ap[0:16, 0:16]), reads=[affT, self.identf], writes=[psG])
            P.op("act", lambda e, NC_=NC_: e.copy(out=G.ap[:, 0:NC_], in_=psG.ap[:, 0:NC_]), reads=[psG], writes=[G])
            P.op("dve", lambda e, NC_=NC_: e.tensor_scalar(out=mask.ap[:, 0:NC_], in0=G.ap[:, 0:NC_], scalar1=0.0, scalar2=None, op0=ALU.is_gt),
                 reads=[G], writes=[mask])
            psPos = P.psum[7]; psCs = P.psum[5]
            P.op("pe", lambda e, NC_=NC_: e.matmul(psPos.ap[:, 0:NC_], lhsT=self.cb.ap[:, 2, :], rhs=mask.ap[:, 0:NC_], start=True, stop=True),
                 reads=[self.cb, mask], writes=[psPos])
            P.op("pe", lambda e, NC_=NC_: e.matmul(psCs.ap[:, 0:NC_], lhsT=self.cb.ap[:, 1, :], rhs=mask.ap[:, 0:NC_], start=True, stop=True),
                 reads=[self.cb, mask], writes=[psCs])
            P.op("act", lambda e, NC_=NC_: e.copy(out=CS.ap[:, 0:NC_], in_=psCs.ap[:, 0:NC_]), reads=[psCs], writes=[CS])
            P.op("dve", lambda e: e.memset(Pfx.ap[:, 0:16], 0.0), writes=[Pfx])
            for i in range(1, nblk):
                P.op("dve", lambda e, i=i: e.tensor_tensor(out=Pfx.ap[:, i * 16:(i + 1) * 16], in0=Pfx.ap[:, (i - 1) * 16:i * 16],
                                                           in1=CS.ap[:, (i - 1) * 16:i * 16], op=ALU.add), reads=[Pfx, CS], writes=[Pfx])
            P.op("dve", lambda e, NC_=NC_: e.tensor_tensor(out=posm.ap[:, 0:NC_], in0=psPos.ap[:, 0:NC_], in1=Pfx.ap[:, 0:NC_], op=ALU.add),
                 reads=[psPos, Pfx], writes=[posm])
            P.op("dve", lambda e, NC_=NC_: e.scalar_tensor_tensor(out=posm.ap[:, 0:NC_], in0=posm.ap[:, 0:NC_], scalar=1.0, in1=mask.ap[:, 0:NC_],
                                                                 op0=ALU.add, op1=ALU.mult), reads=[posm, mask], writes=[posm])
            P.op("dve", lambda e, NC_=NC_: e.tensor_scalar(out=posm.ap[:, 0:NC_], in0=posm.ap[:, 0:NC_], scalar1=-1.0, scalar2=None, op0=ALU.add),
                 reads=[posm], writes=[posm])
            P.op("dve", lambda e, NC_=NC_: e.tensor_copy(out=ghi.ap[:, 0:NC_], in_=G.ap[:, 0:NC_]), reads=[G], writes=[ghi])
            P.op("dve", lambda e, NC_=NC_: e.tensor_tensor(out=r1.ap[:, 0:NC_], in0=G.ap[:, 0:NC_], in1=ghi.ap[:, 0:NC_], op=ALU.subtract),
                 reads=[G, ghi], writes=[r1])
            P.op("dve", lambda e, NC_=NC_: e.tensor_copy(out=gmid.ap[:, 0:NC_], in_=r1.ap[:, 0:NC_]), reads=[r1], writes=[gmid])
            v4 = lambda t, NC_=NC_: t.ap[:, 0:NC_].rearrange("p (b e) -> p b e", e=16)
            P.op("dve", lambda e, o=vals.ap[:, 0:nblk, :, 2], i=v4(ghi): e.tensor_copy(out=o, in_=i), reads=[ghi], writes=[vals])
            P.op("dve", lambda e, o=vals.ap[:, 0:nblk, :, 3], i=v4(gmid): e.tensor_copy(out=o, in_=i), reads=[gmid], writes=[vals])
            P.op("dve", lambda e, NC_=NC_: e.tensor_tensor(out=r1.ap[:, 0:NC_], in0=r1.ap[:, 0:NC_], in1=gmid.ap[:, 0:NC_], op=ALU.subtract),
                 reads=[r1, gmid], writes=[r1])
            P.op("dve", lambda e, o=vals.ap[:, 0:nblk, :, 4], i=v4(r1): e.tensor_copy(out=o, in_=i), reads=[r1], writes=[vals])
            b0 = s0 // 128
            for ee in range(NEXP):
                P.op("dve", lambda e, o=vals.ap[:, 0:nblk, ee, 0:2], i=self.tokv.ap[:, b0:b0 + nblk, :]: e.tensor_copy(out=o, in_=i),
                     reads=[self.tokv], writes=[vals])
            npart = min(cap, 128)
            nsg = (cap + 127) // 128
            v3 = lambda t, NC_=NC_: t.ap[:, 0:NC_].rearrange("p (b e) -> p b e", e=16)
            if nsg > 1:
                P.op("dve", lambda e, NC_=NC_: e.tensor_scalar(out=sgf.ap[:, 0:NC_], in0=posm.ap[:, 0:NC_], scalar1=128.0, scalar2=None, op0=ALU.is_ge),
                     reads=[posm], writes=[sgf])
                for thr in (256.0, 384.0):
                    P.op("dve", lambda e, NC_=NC_, thr=thr: e.scalar_tensor_tensor(out=sgf.ap[:, 0:NC_], in0=posm.ap[:, 0:NC_], scalar=thr,
                                                                                  in1=sgf.ap[:, 0:NC_], op0=ALU.is_ge, op1=ALU.add),
                         reads=[posm, sgf], writes=[sgf])
                P.op("dve", lambda e, NC_=NC_: e.scalar_tensor_tensor(out=posm.ap[:, 0:NC_], in0=sgf.ap[:, 0:NC_], scalar=-128.0, in1=posm.ap[:, 0:NC_],
                                                                     op0=ALU.mult, op1=ALU.add), reads=[posm, sgf], writes=[posm])
                for sg in range(nsg):
                    P.op("dve", lambda e, NC_=NC_, sg=sg: e.tensor_scalar(out=oh.ap[:, 0:NC_], in0=sgf.ap[:, 0:NC_], scalar1=float(sg), scalar2=None,
                                                                         op0=ALU.is_equal), reads=[sgf], writes=[oh])
                    for c in range(5):
                        P.op("dve", lambda e, o=vals4.ap[:, 0:nblk, :, sg, c], i0_=vals.ap[:, 0:nblk, :, c], i1_=v3(oh): e.tensor_tensor(
                            out=o, in0=i0_, in1=i1_, op=ALU.mult), reads=[vals, oh], writes=[vals4])
            else:
                for c in range(5):
                    P.op("dve", lambda e, o=vals4.ap[:, 0:nblk, :, 0, c], i0_=vals.ap[:, 0:nblk, :, c]: e.tensor_copy(out=o, in_=i0_),
                         reads=[vals], writes=[vals4])
            iota_bc = self.iota.ap[:, 0:npart].unsqueeze(1).broadcast_to([128, nblk, npart])
            pos3 = posm.ap[:, 0:NC_].rearrange("p (b e) -> p b e", e=16)
            for ee in range(NEXP):
                ps = P.psum[ee % 4]
                sel = selB[ee % 2]
                en = "dve"
                P.op(en, lambda e, o=sel.ap[:, 0:nblk, 0:npart], i0_=iota_bc, i1_=pos3[:, :, ee].unsqueeze(2).broadcast_to([128, nblk, npart]):
                     e.tensor_tensor(out=o, in0=i0_, in1=i1_, op=ALU.is_equal), reads=[self.iota, posm], writes=[sel])
                for blk in range(nblk):
                    P.op("pe", lambda e, ps=ps, sel=sel, blk=blk, ee=ee, npart=npart, nsg=nsg, last=(blk == nblk - 1): e.matmul(
                        ps.ap[0:npart, 0:nsg * 5], lhsT=sel.ap[:, blk, 0:npart], rhs=vals4.ap[:, blk, ee, 0:nsg, :],
                        start=(blk == 0), stop=last), reads=[sel, vals4], writes=[ps])
                col0 = ee * 4 if s == 0 else 64 + ee
                P.op("act", lambda e, ps=ps, npart=npart, nsg=nsg: e.copy(out=iv.ap[0:npart, 0:nsg * 5], in_=ps.ap[0:npart, 0:nsg * 5]),
                     reads=[ps], writes=[iv])
                iv5 = iv.ap[0:npart, 0:nsg * 5].rearrange("p (g c) -> p g c", c=5)
                P.op("dve", lambda e, iv5=iv5, npart=npart, nsg=nsg: e.scalar_tensor_tensor(out=iv2.ap[0:npart, 0:nsg], in0=iv5[:, :, 0], scalar=64.0,
                                                                                       in1=iv5[:, :, 1], op0=ALU.mult, op1=ALU.add), reads=[iv], writes=[iv2])
                P.op("dve", lambda e, npart=npart, nsg=nsg, col0=col0: e.tensor_copy(out=self.IDX.ap[0:npart, col0:col0 + nsg], in_=iv2.ap[0:npart, 0:nsg]),
                     reads=[iv2], writes=[self.IDX])
                P.op("dve", lambda e, iv5=iv5, npart=npart, nsg=nsg: e.tensor_tensor(out=iv2.ap[0:npart, 4:4 + nsg], in0=iv5[:, :, 2], in1=iv5[:, :, 3],
                                                                                op=ALU.add), reads=[iv], writes=[iv2])
                P.op("dve", lambda e, iv5=iv5, npart=npart, nsg=nsg, col0=col0: e.tensor_tensor(out=self.GV.ap[0:npart, col0:col0 + nsg], in0=iv2.ap[0:npart, 4:4 + nsg],
                                                                                           in1=iv5[:, :, 4], op=ALU.add), reads=[iv, iv2], writes=[self.GV])
        if self.debug:
            idxd = self.nc.dram_tensor("IDXD", [128, 80], I32, kind="ExternalOutput").ap()
            gvd = self.nc.dram_tensor("GVD", [128, 80], F32, kind="ExternalOutput").ap()
            P.dma("sp", idxd[:, :], self.IDX.ap[:], reads=[self.IDX])
            P.dma("sp", gvd[:, :], self.GV.ap[:], reads=[self.GV])

    def phase_expert(self, l):
        P, I = self.P, self.i
        P.phase_begin()
        sets = [(0, 512, 128, 4)] + ([(1, 32, 32, 1)] if l == 0 else [])
        g2 = {s: self.load_mod("g2_%d" % s, s, M_G2) for (s, _, _, _) in sets}
        P.top_off = self._pre_top
        wb = [self._pre_wb0,
              [P.sb("w%s1" % n, [128, 8, D], BF16) for n in ("g", "u", "d")]]
        xes = [[P.sb("xe%d_%d" % (i, j), [128, D], BF16) for j in range(5)] for i in range(2)]
        xeT = P.sb("xeT", [128, 8, 512], BF16)
        sas = [P.sb("sae%d" % i, [128, 512], F32) for i in range(2)]
        hm = P.sb("hm", [128, 8, 512], BF16)
        yes = [P.sb("ye%d" % i, [128, D], F32) for i in range(2)]

        def loadw(e):
            for n, nm in enumerate(("w_e_gate", "w_e_up", "w_e_down")):
                self.load_cast_rows(wb[e % 2][n], I[nm][l, e], D, None, 8)

        def jobs(e):
            out = []
            for (s, nsel, npart, ngrp) in sets:
                for sg in range(ngrp):
                    col = (e * 4 + sg) if s == 0 else (64 + e)
                    out.append((s, sg, col, npart, xes[e % 2][sg if s == 0 else 4]))
            return out

        def gather(e):
            for (s, sg, col, npart, xe) in jobs(e):
                P._dma("pool", lambda q, xe=xe, npart=npart, col=col: q.indirect_dma_start(
                    out=xe.ap[0:npart, :], out_offset=None, in_=self.H2[:, :],
                    in_offset=bass.IndirectOffsetOnAxis(ap=self.IDX.ap[0:npart, col:col + 1], axis=0)),
                    [self.IDX, self.rH2], [xe])

        gather(0)
        ny = 0; nsa = 0; ntr = 0
        for e_ in range(NEXP):
            if e_ + 1 < NEXP:
                loadw(e_ + 1)
                gather(e_ + 1)
            wg, wu, wd = wb[e_ % 2]
            for (s, nsel, npart, ngrp) in sets:
                for sg in range(ngrp):
                    xe = xes[e_ % 2][sg if s == 0 else 4]
                    self.transpose8(xe, P.psum[ntr % 2], xeT.ap[:, :, sg * npart:(sg + 1) * npart], xeT, nrows=npart,
                                    eng=("act" if ntr % 2 else "dve"))
                    ntr += 1
                for fc in range(8):
                    psA = P.psum[2 + (fc % 2) * 2]; psU = P.psum[3 + (fc % 2) * 2]
                    for (ps, w_) in ((psA, wg), (psU, wu)):
                        for k in range(8):
                            P.op("pe", lambda e, ps=ps, w_=w_, k=k, fc=fc, nsel=nsel: e.matmul(
                                ps.ap[:, 0:nsel], lhsT=w_.ap[:, k, fc * 128:(fc + 1) * 128], rhs=xeT.ap[:, k, 0:nsel],
                                start=(k == 0), stop=(k == 7)), reads=[w_.part(k), xeT], writes=[ps])
                    sa = sas[nsa % 2]; nsa += 1
                    P.op("act", lambda e, psA=psA, nsel=nsel, sa=sa: e.activation(out=sa.ap[:, 0:nsel], in_=psA.ap[:, 0:nsel], func=AF.Silu),
                         reads=[psA], writes=[sa])
                    P.op("dve", lambda e, psU=psU, fc=fc, nsel=nsel, sa=sa: e.tensor_tensor(out=hm.ap[:, fc, 0:nsel], in0=psU.ap[:, 0:nsel], in1=sa.ap[:, 0:nsel],
                                                                                       op=ALU.mult), reads=[psU, sa], writes=[hm])
                for sg in range(ngrp):
                    col = (e_ * 4 + sg) if s == 0 else (64 + e_)
                    ye = yes[ny % 2]; ny += 1
                    for half in range(2):
                        ps = P.psum[6 + half]
                        for fc in range(8):
                            P.op("pe", lambda e, ps=ps, fc=fc, sg=sg, half=half, npart=npart, wd=wd: e.matmul(
                                ps.ap[0:npart, :], lhsT=hm.ap[:, fc, sg * npart:(sg + 1) * npart], rhs=wd.ap[:, fc, half * 512:(half + 1) * 512],
                                start=(fc == 0), stop=(fc == 7)), reads=[hm, wd.part(fc)], writes=[ps])
                        P.op("dve", lambda e, ps=ps, ye=ye, half=half, npart=npart, col=col, s=s: e.scalar_tensor_tensor(
                            out=ye.ap[0:npart, half * 512:(half + 1) * 512], in0=ps.ap[0:npart, :], scalar=self.GV.ap[0:npart, col:col + 1],
                            in1=g2[s].ap[0:npart, half * 512:(half + 1) * 512], op0=ALU.mult, op1=ALU.mult),
                            reads=[ps, self.GV, g2[s]], writes=[ye])
                    P._dma("pool", lambda q, ye=ye, npart=npart, col=col: q.indirect_dma_start(
                        out=self.XR[:, :], out_offset=bass.IndirectOffsetOnAxis(ap=self.IDX.ap[0:npart, col:col + 1], axis=0),
                        in_=ye.ap[0:npart, :], in_offset=None, compute_op=ALU.add, oob_is_err=True),
                        [ye, self.IDX], [self.rXRS])

    def phase_final(self):
        P, I = self.P, self.i
        P.phase_begin()
        fg = P.sb("fg", [128, D], F32)
        P.dma("sp", fg.ap[:], I["fng"][:, :], writes=[fg])
        xts = [P.sb("xf%d" % i, [128, D], F32) for i in range(2)]
        obs = [P.sb("of%d" % i, [128, D], F32) for i in range(2)]
        junk = P.sb("junkf", [128, D], BF16)
        ss = P.sb("ssf", [128, 2], F32)
        for i in range(32):
            xt = xts[i % 2]; ob = obs[i % 2]
            rx = list(self.blk(self.rXR, i * 128, (i + 1) * 128)) + [self.rXRS]
            P.dma("sp", xt.ap[:], self.XR[i * 128:(i + 1) * 128, :], reads=rx, writes=[xt])
            self.norm_mod(xt, fg, None, ob, ss, junk)
            P.dma("pool", self.out[i * 128:(i + 1) * 128, :], ob.ap[:], reads=[ob], writes=[])


def _bf(a):
    return np.asarray(a, np.float32).astype(ml_dtypes.bfloat16)


def host_consts():
    c = {}
    cb = np.zeros((128, 7, 128), np.float32)
    cb[:, 0, :] = np.eye(128)
    cb[:, 1, :] = 1.0
    tp = np.arange(128)
    cb[:, 2, :] = (tp[:, None] < tp[None, :]).astype(np.float32)
    cb[:, 3, :] = 1.0 / 512
    c["cb16"] = _bf(cb)
    mk = np.zeros((128, 2, 512), np.float32)
    j = np.arange(128)[:, None]; i = np.arange(128)[None, :]
    mk[:, 0, :] = np.tile((j >= i).astype(np.float32), (1, 4))
    mk[:, 1, :] = np.tile((j <= i).astype(np.float32), (1, 4))
    c["mk4"] = _bf(mk)
    c["identf"] = np.eye(128, dtype=np.float32)
    c["iota"] = np.tile(np.arange(512, dtype=np.float32)[None, :], (128, 1))
    t = (np.arange(34)[None, :] * 128 + np.arange(128)[:, None])
    c["tokv"] = _bf(np.stack([t // 64, t % 64], axis=-1))
    tt = np.arange(S)
    row = (tt // 64).astype(np.float64); col = (tt % 64).astype(np.float64)
    freqs = (10000.0 ** (-np.arange(16, dtype=np.float32) / np.float32(16))).astype(np.float32).astype(np.float64)
    ar = row[None, :] * freqs[:, None]; ac = col[None, :] * freqs[:, None]
    cos = np.concatenate([np.cos(ar), np.cos(ar), np.cos(ac), np.cos(ac)], 0)
    sin = np.concatenate([-np.sin(ar), np.sin(ar), -np.sin(ac), np.sin(ac)], 0)
    rope = np.zeros((4, 64, NT), np.float32)
    rope[0, :, :S] = cos; rope[0, :, S:] = 1.0
    rope[1, :, :S] = sin
    rope[2, :, :S] = cos / 8; rope[2, :, S:] = 1.0 / 8
    rope[3, :, :S] = sin / 8
    c["rope"] = rope
    invc = np.zeros((4, NT), np.float32)
    for gi, w in enumerate((2, 4, 8, 16)):
        for (s0, N) in ((0, S), (S, L)):
            tl = np.arange(N)
            lo = np.clip(tl - w // 2, 0, N); hi = np.clip(tl + w - w // 2, 0, N)
            invc[gi, s0:s0 + N] = (1.0 / (hi - lo).astype(np.float32))
    c["invc"] = np.ascontiguousarray(np.broadcast_to(invc[None], (128, 4, NT)))
    return c


def host_inputs(inp, b, consts):
    f = lambda a: np.ascontiguousarray(np.asarray(a, np.float32))
    rep = lambda v: np.ascontiguousarray(np.broadcast_to(np.asarray(v, np.float32)[None, :], (128, v.shape[-1])))
    pk = lambda v, n: np.ascontiguousarray(np.asarray(v, np.float32).reshape(n, 128).T)
    m = dict(consts)
    m["x"] = f(inp["x"][b]); m["ctx"] = f(inp["ctx"][b])
    m["cvec"] = np.ascontiguousarray(np.stack([pk(inp["c"][b], 8), pk(inp["c_ctx"], 8)], -1))
    m["ng"] = np.stack([np.stack([rep(inp["norm1_g"][l]), rep(inp["norm2_g"][l])]) for l in range(2)])
    m["fng"] = rep(inp["final_norm_g"])
    m["w_mod"] = f(inp["w_mod"]); m["bmod"] = np.stack([rep(inp["b_mod"][l]) for l in range(2)])
    m["w_in"] = f(inp["w_in"])
    m["sink"] = np.stack([rep(inp["attn_sink"][l]) for l in range(2)])
    m["w_attn_o"] = f(inp["w_attn_o"])
    m["dwt"] = np.stack([np.ascontiguousarray(np.asarray(inp["conv_dw"][l], np.float32).T.reshape(4, 128, 31).transpose(1, 0, 2))
                         for l in range(2)])
    m["cvp"] = np.stack([np.stack([pk(inp["conv_dw_b"][l], 4), pk(inp["conv_ln_g"][l], 4), pk(inp["conv_ln_b"][l], 4)], 1)
                         for l in range(2)])
    m["w_conv_o"] = f(inp["w_conv_o"]); m["w_pool"] = f(inp["w_pool"])
    m["psc"] = np.stack([pk(inp["pool_scale"][l], 4) for l in range(2)])
    m["w_pool_o"] = f(inp["w_pool_o"]); m["w_out"] = f(inp["w_out"]); m["w_router"] = f(inp["w_router"])
    m["w_e_gate"] = f(inp["w_e_gate"]); m["w_e_up"] = f(inp["w_e_up"]); m["w_e_down"] = f(inp["w_e_down"])
    return m


_CACHE = {}
NCORES = 4


def build(phases=None, debug=False):
    k = K(debug=debug)
    k.setup()
    for l in range(2):
        for name in ("mod", "inproj", "mix", "outp", "route", "expert"):
            if phases is not None and (l, name) not in phases:
                continue
            getattr(k, "phase_" + name)(l)
    if phases is None or "final" in phases:
        k.phase_final()
    k.P.finish()
    return k


def kernel(**inputs):
    inp = {n: np.asarray(v) for n, v in inputs.items()}
    if "k" not in _CACHE:
        _CACHE["k"] = build()
        _CACHE["c"] = host_consts()
    k = _CACHE["k"]
    maps = [host_inputs(inp, c % 4, _CACHE["c"]) for c in range(NCORES)]
    res = run_bass_kernel_spmd(k.nc, maps, core_ids=list(range(NCORES)))
    out = np.stack([np.asarray(res.results[b]["out"], np.float32) for b in range(4)])
    return out
```
